# Optimizing a Trainium2 kernel written in Bass

```python
import jax, jax.numpy as jnp
from jax import lax
import numpy as np

D_MODEL = 2048
BATCH = 4
SEQ = 2048
DEPTH = 2

HEAD_DIM = 64
BLOCK = 128
ROPE_THETA = 10000.0
NORM_EPS = 1e-6

DIL_PATTERNS = ((128, 1), (512, 4), (2048, 16))
N_DIL = len(DIL_PATTERNS)
A_HEADS = 8
B_Q_HEADS = 8
B_KV_HEADS = 2
B_WINDOW = 128
C_HEADS = 8
D_HEADS = 8
D_NOPE = 64
D_ROPE = 32
D_V = 64
Q_LORA = 384
KV_LORA = 256

N_BRANCH = 4
BRANCH_WIDTH = 512

D_FF = 5632
N_EXPERTS = 8
TOP_K = 2
D_FF_EXPERT = 7168
N_DENSE = (DEPTH + 1) // 2
N_MOE = DEPTH // 2

A_W = N_DIL * A_HEADS * HEAD_DIM
B_QW = B_Q_HEADS * HEAD_DIM
B_KW = B_KV_HEADS * HEAD_DIM
C_W = C_HEADS * HEAD_DIM
IN_SPLITS = (A_W, A_W, A_W, B_QW, B_KW, B_KW, C_W, C_W, C_W, Q_LORA, KV_LORA, D_ROPE, N_BRANCH * D_MODEL)
IN_WIDTH = sum(IN_SPLITS)
SPLIT_POINTS = tuple(int(v) for v in np.cumsum(IN_SPLITS)[:-1])

kernel_name = 'hybrid_dilated_sink_stickbreak_mla_moe'

F32 = jnp.float32


def rmsnorm(x, g):
    xf = x.astype(F32)
    y = xf * lax.rsqrt(jnp.mean(xf * xf, axis=-1, keepdims=True) + NORM_EPS)
    return (y * g.astype(F32)).astype(x.dtype)


def rope(x, pos):
    dh = x.shape[-1]
    half = dh // 2
    freqs = ROPE_THETA ** (-2.0 * jnp.arange(half, dtype=F32) / dh)
    ang = pos.astype(F32)[:, None] * freqs[None, :]
    cos = jnp.cos(ang)[:, None, :]
    sin = jnp.sin(ang)[:, None, :]
    xf = x.astype(F32)
    x1, x2 = xf[..., :half], xf[..., half:]
    return jnp.concatenate([x1 * cos - x2 * sin, x2 * cos + x1 * sin], axis=-1).astype(x.dtype)


def to_strided(t, d):
    B, S = t.shape[:2]
    rest = t.shape[2:]
    return t.reshape(B, S // d, d, *rest).swapaxes(1, 2).reshape(B * d, S // d, *rest)


def from_strided(t, d, B):
    L = t.shape[1]
    rest = t.shape[2:]
    return t.reshape(B, d, L, *rest).swapaxes(1, 2).reshape(B, L * d, *rest)


def banded_attention(q, k, v, max_dist, sinks=None):
    N, L, Hq, dh = q.shape
    Hkv = k.shape[2]
    G = Hq // Hkv
    nb = -(-L // BLOCK)
    pad = nb * BLOCK - L
    padw = ((0, 0), (0, pad), (0, 0), (0, 0))
    q = jnp.pad(q, padw)
    k = jnp.pad(k, padw)
    v = jnp.pad(v, padw)
    qb = q.reshape(N, nb, BLOCK, Hkv, G, dh)

    def window(t):
        tb = t.reshape(N, nb, BLOCK, Hkv, dh)
        prev = jnp.pad(tb, ((0, 0), (1, 0), (0, 0), (0, 0), (0, 0)))[:, :nb]
        return jnp.concatenate([prev, tb], axis=2)

    kw, vw = window(k), window(v)
    s = jnp.einsum('nbqhgd,nbkhd->nbhgqk', qb, kw, preferred_element_type=F32) * (dh ** -0.5)
    qpos = jnp.arange(nb)[:, None, None] * BLOCK + jnp.arange(BLOCK)[None, :, None]
    kpos = jnp.arange(nb)[:, None, None] * BLOCK - BLOCK + jnp.arange(2 * BLOCK)[None, None, :]
    dist = qpos - kpos
    valid = (dist >= 0) & (dist <= max_dist) & (kpos >= 0)
    s = jnp.where(valid[None, :, None, None], s, -jnp.inf)
    m = s.max(axis=-1)
    if sinks is not None:
        sk = sinks.astype(F32).reshape(Hkv, G)[None, None, :, :, None]
        m = jnp.maximum(m, sk)
    p = jnp.exp(s - m[..., None])
    denom = p.sum(axis=-1)
    if sinks is not None:
        denom = denom + jnp.exp(sk - m)
    o = jnp.einsum('nbhgqk,nbkhd->nbqhgd', p, vw.astype(F32)) / jnp.moveaxis(denom, -1, 2)[..., None]
    lse = jnp.moveaxis(m + jnp.log(denom), -1, 2).reshape(N, nb * BLOCK, Hq)[:, :L]
    o = o.reshape(N, nb * BLOCK, Hq, dh)[:, :L]
    return o, lse


def dilated_attention(q, k, v, pos):
    B, S = q.shape[:2]
    q = rope(q, pos)
    k = rope(k, pos)
    outs, lses = [], []
    for g, (win, dil) in enumerate(DIL_PATTERNS):
        hs = slice(g * A_HEADS, (g + 1) * A_HEADS)
        o, lse = banded_attention(to_strided(q[:, :, hs], dil), to_strided(k[:, :, hs], dil),
                                  to_strided(v[:, :, hs], dil), win // dil)
        outs.append(from_strided(o, dil, B))
        lses.append(from_strided(lse, dil, B))
    w = jax.nn.softmax(jnp.stack(lses), axis=0)
    o = jnp.einsum('gbsh,gbshd->bshd', w, jnp.stack(outs))
    return o.reshape(B, S, A_HEADS * HEAD_DIM).astype(q.dtype)


def sliding_sink_attention(q, k, v, sinks, pos):
    B, S = q.shape[:2]
    o, _ = banded_attention(rope(q, pos), rope(k, pos), v, B_WINDOW - 1, sinks)
    return o.reshape(B, S, B_Q_HEADS * HEAD_DIM).astype(q.dtype)


def stick_breaking_attention(q, k, v):
    B, S, H, dh = q.shape
    nb = S // BLOCK
    qb = q.reshape(B, nb, BLOCK, H, dh).swapaxes(0, 1)
    kpos = jnp.arange(S)
    scale = dh ** -0.5
    vf = v.astype(F32)

    def block(args):
        q_blk, b = args
        z = jnp.einsum('bqhd,bkhd->bhqk', q_blk, k, preferred_element_type=F32) * scale
        qpos = b * BLOCK + jnp.arange(BLOCK)
        before = (kpos[None, :] < qpos[:, None])[None, None]
        log_stay = jnp.where(before, jax.nn.log_sigmoid(-z), 0.0)
        log_after = lax.cumsum(log_stay, axis=3, reverse=True) - log_stay
        a = jnp.where(before, jnp.exp(jax.nn.log_sigmoid(z) + log_after), 0.0)
        return jnp.einsum('bhqk,bkhd->bqhd', a, vf)

    o = lax.map(block, (qb, jnp.arange(nb)))
    return o.swapaxes(0, 1).reshape(B, S, H * dh).astype(q.dtype)


def latent_attention(c_q, c_kv, k_rope, g_qa, g_kva, w_uq, w_ukv, pos):
    B, S = c_q.shape[:2]
    q = (rmsnorm(c_q, g_qa) @ w_uq).reshape(B, S, D_HEADS, D_NOPE + D_ROPE)
    q_nope, q_rope = q[..., :D_NOPE], rope(q[..., D_NOPE:], pos)
    kv = (rmsnorm(c_kv, g_kva) @ w_ukv).reshape(B, S, D_HEADS, D_NOPE + D_V)
    k_nope, v = kv[..., :D_NOPE], kv[..., D_NOPE:].astype(F32)
    k_r = rope(k_rope[:, :, None, :], pos)[:, :, 0]
    scale = (D_NOPE + D_ROPE) ** -0.5
    nb = S // BLOCK
    qn = q_nope.reshape(B, nb, BLOCK, D_HEADS, D_NOPE).swapaxes(0, 1)
    qr = q_rope.reshape(B, nb, BLOCK, D_HEADS, D_ROPE).swapaxes(0, 1)
    kpos = jnp.arange(S)

    def block(args):
        qn_b, qr_b, b = args
        s = (jnp.einsum('bqhd,bkhd->bhqk', qn_b, k_nope, preferred_element_type=F32)
             + jnp.einsum('bqhr,bkr->bhqk', qr_b, k_r, preferred_element_type=F32)) * scale
        qpos = b * BLOCK + jnp.arange(BLOCK)
        s = jnp.where(kpos[None, :] <= qpos[:, None], s, -jnp.inf)
        p = jax.nn.softmax(s, axis=-1)
        return jnp.einsum('bhqk,bkhd->bqhd', p, v)

    o = lax.map(block, (qn, qr, jnp.arange(nb)))
    return o.swapaxes(0, 1).reshape(B, S, D_HEADS * D_V).astype(c_q.dtype)


def mixer_block(n, pos, w_in, w_branch, w_out, sinks, g_qa, g_kva, w_uq, w_ukv):
    B, S, _ = n.shape
    proj = n @ w_in
    (aq, ak, av, bq, bk, bv, cq, ck, cv, dcq, dckv, dkr, gates) = jnp.split(proj, SPLIT_POINTS, axis=-1)
    heads = lambda t: t.reshape(B, S, -1, HEAD_DIM)
    o_a = dilated_attention(heads(aq), heads(ak), heads(av), pos)
    o_b = sliding_sink_attention(heads(bq), heads(bk), heads(bv), sinks, pos)
    o_c = stick_breaking_attention(heads(cq), heads(ck), heads(cv))
    o_d = latent_attention(dcq, dckv, dkr, g_qa, g_kva, w_uq, w_ukv, pos)
    branches = jnp.stack([o_a, o_b, o_c, o_d], axis=2)
    g = jax.nn.sigmoid(gates.reshape(B, S, N_BRANCH, D_MODEL).astype(F32))
    merged = (jnp.einsum('bsne,ned->bsnd', branches, w_branch, preferred_element_type=F32) * g).sum(axis=2)
    return merged.astype(n.dtype) @ w_out


def swiglu(x, w_gate, w_up, w_down):
    return (jax.nn.silu(x @ w_gate) * (x @ w_up)) @ w_down


def moe_swiglu(x, w_router, w_gate, w_up, w_down):
    B, S, D = x.shape
    xt = x.reshape(B * S, D)
    logits = (xt @ w_router).astype(F32)
    top_val, top_idx = lax.top_k(logits, TOP_K)
    top_w = jax.nn.softmax(top_val, axis=-1)
    combine = jnp.einsum('tk,tke->te', top_w, jax.nn.one_hot(top_idx, N_EXPERTS, dtype=F32))
    out = sum(combine[:, e, None] * swiglu(xt, w_gate[e], w_up[e], w_down[e]) for e in range(N_EXPERTS))
    return out.reshape(B, S, D).astype(x.dtype)


def setup_inputs(seed: int = 0) -> dict:
    key = jax.random.key(seed)
    ks = jax.random.split(key, 20)
    nrm = lambda k, shape, fan_in: jax.random.normal(k, shape, F32) * (fan_in ** -0.5)
    gain = lambda k, shape: 1.0 + 0.02 * jax.random.normal(k, shape, F32)
    return {
        'x': jax.random.normal(ks[0], (BATCH, SEQ, D_MODEL), F32),
        'w_in': nrm(ks[1], (DEPTH, D_MODEL, IN_WIDTH), D_MODEL),
        'w_branch': nrm(ks[2], (DEPTH, N_BRANCH, BRANCH_WIDTH, D_MODEL), BRANCH_WIDTH),
        'w_out': nrm(ks[3], (DEPTH, D_MODEL, D_MODEL), D_MODEL),
        'norm_mix': gain(ks[4], (DEPTH, D_MODEL)),
        'norm_ffn': gain(ks[5], (DEPTH, D_MODEL)),
        'norm_final': gain(ks[6], (D_MODEL,)),
        'sinks': 0.5 * jax.random.normal(ks[7], (DEPTH, B_Q_HEADS), F32),
        'mla_q_norm': gain(ks[8], (DEPTH, Q_LORA)),
        'mla_kv_norm': gain(ks[9], (DEPTH, KV_LORA)),
        'mla_w_uq': nrm(ks[10], (DEPTH, Q_LORA, D_HEADS * (D_NOPE + D_ROPE)), Q_LORA),
        'mla_w_ukv': nrm(ks[11], (DEPTH, KV_LORA, D_HEADS * (D_NOPE + D_V)), KV_LORA),
        'ffn_w_gate': nrm(ks[12], (N_DENSE, D_MODEL, D_FF), D_MODEL),
        'ffn_w_up': nrm(ks[13], (N_DENSE, D_MODEL, D_FF), D_MODEL),
        'ffn_w_down': nrm(ks[14], (N_DENSE, D_FF, D_MODEL), D_FF),
        'router_w': nrm(ks[15], (N_MOE, D_MODEL, N_EXPERTS), D_MODEL),
        'moe_w_gate': nrm(ks[16], (N_MOE, N_EXPERTS, D_MODEL, D_FF_EXPERT), D_MODEL),
        'moe_w_up': nrm(ks[17], (N_MOE, N_EXPERTS, D_MODEL, D_FF_EXPERT), D_MODEL),
        'moe_w_down': nrm(ks[18], (N_MOE, N_EXPERTS, D_FF_EXPERT, D_MODEL), D_FF_EXPERT),
    }


def reference(x, w_in, w_branch, w_out, norm_mix, norm_ffn, norm_final, sinks, mla_q_norm, mla_kv_norm,
              mla_w_uq, mla_w_ukv, ffn_w_gate, ffn_w_up, ffn_w_down, router_w, moe_w_gate, moe_w_up, moe_w_down):
    pos = jnp.arange(x.shape[1])
    h = x
    for layer in range(DEPTH):
        n = rmsnorm(h, norm_mix[layer])
        h = h + mixer_block(n, pos, w_in[layer], w_branch[layer], w_out[layer], sinks[layer],
                            mla_q_norm[layer], mla_kv_norm[layer], mla_w_uq[layer], mla_w_ukv[layer])
        n = rmsnorm(h, norm_ffn[layer])
        i = layer // 2
        if layer % 2 == 0:
            h = h + swiglu(n, ffn_w_gate[i], ffn_w_up[i], ffn_w_down[i])
        else:
            h = h + moe_swiglu(n, router_w[i], moe_w_gate[i], moe_w_up[i], moe_w_down[i])
    return rmsnorm(h, norm_final)
```

```python
import numpy as np
from contextlib import ExitStack
import concourse.bass as bass
import concourse.mybir as mybir
from concourse.bass_utils import run_bass_kernel_spmd

F32 = mybir.dt.float32
BF16 = mybir.dt.bfloat16
AF = mybir.ActivationFunctionType
ALU = mybir.AluOpType

S = 2048
T = 1024
NB = 8
EPS = 1e-6
NDMA = 24
COMPUTE = ("pe", "act", "dve", "pool")

QA, QB, QC, QD, QROWS = 0, 1536, 2048, 2560, 3328
KA, KB_, KC_, KD, KROWS = 0, 1536, 1664, 2176, 2944
VA, VB, VC, VD, VCOLS = 0, 1536, 1664, 2176, 2688
W_AQ, W_AK, W_AV, W_BQ, W_BK, W_BV, W_CQ, W_CK, W_CV, W_DCQ, W_DCKV, W_DKR, W_G = (
    0, 1536, 3072, 4608, 5120, 5248, 5376, 5888, 6400, 6912, 7296, 7552, 7584)

C_ID, C_P64, C_PD, C_PR, C_TRI, C_ONES = 0, 1, 2, 3, 4, 5
MASK_NAMES = (["D0", "D1", "C0", "C1", "A2f", "A20", "A21", "B-1", "B0", "B1", "A0-1", "A00", "A01"]
              + ["A1%d" % j for j in (-4, -3, -2, -1, 0, 1)])
C_MASK0 = 6
NCST = C_MASK0 + len(MASK_NAMES)
MIDX = {n: C_MASK0 + i for i, n in enumerate(MASK_NAMES)}


class Buf:
    __slots__ = ("t", "w", "r", "ps")

    def __init__(self, t, ps=False):
        self.t = t
        self.w = None
        self.r = {}
        self.ps = ps

    def __getitem__(self, k):
        return self.t[k]


class KB:
    def __init__(self, nc):
        self.nc = nc
        self.eng = dict(pe=nc.tensor, act=nc.scalar, dve=nc.vector, pool=nc.gpsimd, sp=nc.sync)
        self.sem = {e: nc.alloc_semaphore("s_" + e) for e in COMPUTE}
        self.cnt = {e: 0 for e in COMPUTE}
        self.pend = {e: False for e in COMPUTE}
        self.dsem = [nc.alloc_semaphore("s_dma%d" % i) for i in range(NDMA)]
        self.dcnt = [0] * NDMA
        self.drr = 0
        self.waited = {e: {} for e in self.eng}
        self.csem = nc.alloc_semaphore("s_cc")
        self.ccnt = 0
        self.nid = 0

    def _semh(self, key):
        if key == "cc":
            return self.csem
        return self.sem[key] if isinstance(key, str) else self.dsem[key[1]]

    def _wait(self, e, deps):
        best = {}
        for (k, v) in deps:
            if k == "pe" and e == "pe":
                continue
            if best.get(k, 0) < v:
                best[k] = v
        w = self.waited[e]
        for k, v in best.items():
            if w.get(k, 0) >= v:
                continue
            self.eng[e].wait_ge(self._semh(k), v)
            w[k] = v

    def _deps(self, reads, writes):
        deps = []
        for b in reads:
            if b.w:
                deps.append(b.w)
        for b in writes:
            if b.w:
                deps.append(b.w)
            deps.extend(b.r.items())
        return deps

    def _record(self, ev, reads, writes):
        for b in reads:
            if b.r.get(ev[0], 0) < ev[1]:
                b.r[ev[0]] = ev[1]
        for b in writes:
            b.w = ev
            b.r = {}

    def op(self, e, fn, reads=(), writes=(), sig=True):
        psr = [b for b in reads if b.ps]
        if psr:
            reads = [b for b in reads if not b.ps]
            writes = list(writes) + [b for b in psr if b not in writes]
        self._wait(e, self._deps(reads, writes))
        ins = fn(self.eng[e])
        ev = (e, self.cnt[e] + 1)
        if sig:
            ins.then_inc(self.sem[e], 1)
            self.cnt[e] += 1
            self.pend[e] = False
        else:
            self.pend[e] = True
        self._record(ev, reads, writes)

    def dma(self, q, out, in_, reads=(), writes=()):
        k = self.drr
        self.drr = (k + 1) % NDMA
        deps = self._deps(reads, writes)
        if self.dcnt[k]:
            deps.append((("d", k), self.dcnt[k]))
        self._wait(q, deps)
        ins = self.eng[q].dma_start(out=out, in_=in_)
        self.dcnt[k] += 16
        ins.then_inc(self.dsem[k], 16)
        self._record((("d", k), self.dcnt[k]), reads, writes)

    def allgather(self, in_ap, out_ap, reads=(), writes=()):
        self._wait("pool", self._deps(reads, writes))
        ins = self.nc.gpsimd.collective_compute(
            "AllGather", ALU.bypass, replica_groups=[[0, 1], [2, 3], [4, 5], [6, 7]],
            ins=[in_ap], outs=[out_ap])
        self.ccnt += 1
        ins.then_inc(self.csem, 1)
        self._record(("cc", self.ccnt), reads, writes)

    def barrier(self):
        for e in COMPUTE:
            assert not self.pend[e], e
        evs = [(e, self.cnt[e]) for e in COMPUTE if self.cnt[e]]
        evs += [(("d", k), self.dcnt[k]) for k in range(NDMA) if self.dcnt[k]]
        if self.ccnt:
            evs.append(("cc", self.ccnt))
        for e in self.eng:
            self._wait(e, [x for x in evs if x[0] != e])

    def sb(self, st, shape, dt, name=None):
        self.nid += 1
        return Buf(st.enter_context(self.nc.sbuf_tensor("%s_%d" % (name or "t", self.nid), list(shape), dt)))

    def ps(self, st, shape, dt=F32, name=None):
        self.nid += 1
        return Buf(st.enter_context(self.nc.psum_tensor("%s_%d" % (name or "p", self.nid), list(shape), dt)), ps=True)


class _Stop(Exception):
    pass


class Rot:
    def __init__(self, bufs):
        self.b = bufs
        self.i = 0

    def next(self):
        b = self.b[self.i % len(self.b)]
        self.i += 1
        return b


def build(DM, DFF, DFE, NE=8, depth=2, dbg=None):
    assert DM % 128 == 0
    KC = DM // 128
    CG = min(512, DM)
    NCG = DM // CG
    INW = W_G + 4 * DM
    nc = bass.Bass("TRN2", target_bir_lowering=False)
    kb = KB(nc)

    lite = bool(dbg) and dbg.startswith("n")
    BIGW = ("w_in", "w_branch", "w_out", "ffn_w_gate", "ffn_w_up", "ffn_w_down", "moe_w_gate", "moe_w_up", "moe_w_down")

    def din(name, shape):
        if lite and name in BIGW:
            shape = [1] * len(shape)
        return nc.dram_tensor(name, list(shape), F32, kind="ExternalInput").ap()

    x = din("x", [T, DM])
    w_in = din("w_in", [depth, DM, INW])
    w_branch = din("w_branch", [depth, 4 * 512, DM])
    w_out = din("w_out", [depth, DM, DM])
    norm_mix = din("norm_mix", [depth, DM])
    norm_ffn = din("norm_ffn", [depth, DM])
    norm_final = din("norm_final", [1, DM])
    sinks = din("sinks", [depth, 8])
    mla_q_norm = din("mla_q_norm", [depth, 384])
    mla_kv_norm = din("mla_kv_norm", [depth, 256])
    mla_w_uq = din("mla_w_uq", [depth, 384, 768])
    mla_w_ukv = din("mla_w_ukv", [depth, 256, 1024])
    ffn_w_gate = din("ffn_w_gate", [1, DM, DFF])
    ffn_w_up = din("ffn_w_up", [1, DM, DFF])
    ffn_w_down = din("ffn_w_down", [1, DFF, DM])
    router_w = din("router_w", [1, DM, NE])
    moe_w_gate = din("moe_w_gate", [NE, DM, DFE])
    moe_w_up = din("moe_w_up", [NE, DM, DFE])
    moe_w_down = din("moe_w_down", [NE, DFE, DM])
    cst_d = din("cst", [128, NCST, 128])
    tab_d = din("tab", [128, 6, T])
    y = nc.dram_tensor("y", [T, DM], F32, kind="ExternalOutput").ap()
    dbg_out = {}

    h_d = nc.dram_tensor("h_d", [T, DM], F32).ap()
    qT_d = nc.dram_tensor("qT_d", [QROWS, T], BF16).ap()
    K_CH = [(0, 1024), (1024, 2048), (2048, KROWS)]
    V_CH = [(0, 3), (3, 6), (6, 8)]
    kin_c = [nc.dram_tensor("kin%d" % j, [b - a, T], BF16).ap() for j, (a, b) in enumerate(K_CH)]
    kout_c = [nc.dram_tensor("kout%d" % j, [2 * (b - a), T], BF16).ap() for j, (a, b) in enumerate(K_CH)]
    vin_c = [nc.dram_tensor("vin%d" % j, [(b - a) * 128, VCOLS], BF16).ap() for j, (a, b) in enumerate(V_CH)]
    vout_c = [nc.dram_tensor("vout%d" % j, [2 * (b - a) * 128, VCOLS], BF16).ap() for j, (a, b) in enumerate(V_CH)]

    def kin_rows(r0, m):
        for j, (a, b) in enumerate(K_CH):
            if a <= r0 and r0 + m <= b:
                return kin_c[j][r0 - a:r0 - a + m, :]
        raise AssertionError((r0, m))

    def kout_rows(r, r0, m):
        for j, (a, b) in enumerate(K_CH):
            if a <= r0 and r0 + m <= b:
                o = r * (b - a) + r0 - a
                return kout_c[j][o:o + m, :]
        raise AssertionError((r0, m))

    def vin_blk(i):
        for j, (a, b) in enumerate(V_CH):
            if a <= i < b:
                return vin_c[j][(i - a) * 128:(i - a + 1) * 128, :]

    def vout_blk(r, i):
        for j, (a, b) in enumerate(V_CH):
            if a <= i < b:
                o = r * (b - a) * 128 + (i - a) * 128
                return vout_c[j][o:o + 128, :]

    def gd_blk(i):
        return g_d[i * 128:(i + 1) * 128, :]

    def qdst(r0):
        return lambda m, tk: qT_d[r0:r0 + m, tk]

    def kdst(r0):
        return lambda m, tk: kin_rows(r0, m)[:, tk]
    g_d = nc.dram_tensor("g_d", [T, 4 * DM], BF16).ap()
    obr_d = nc.dram_tensor("obr_d", [T, 2048], BF16).ap()
    mT_d = nc.dram_tensor("mT_d", [DM, T], BF16).ap()
    D_obr, D_mT = Buf(None), Buf(None)
    D_h, D_q, D_kin, D_kout, D_vin, D_vout, D_g = (Buf(None) for _ in range(7))

    top = ExitStack()
    cst = kb.sb(top, [128, NCST, 128], BF16, "cst")
    identf = kb.sb(top, [128, 128], F32, "identf")
    kb.dma("pool", cst.t[:], cst_d, writes=[cst])
    kb.dma("sp", identf.t[:], cst_d[:, C_ID, :], writes=[identf])

    def cm(idx, m=128):
        return cst.t[0:m, idx, 0:m]

    def rms_to_T(st, src, gB, nT, i, rotp, tmp, router=None):
        srcbuf, srcap = src
        junk, ss, rstd, nb = tmp
        kb.op("dve", lambda e: e.memset(ss.t[:], 0.0), writes=[ss])
        kb.op("act", lambda e: e.activation(out=junk.t[:], in_=srcap, func=AF.Square, accum_out=ss.t[:]),
              reads=[srcbuf], writes=[junk, ss])
        ck("n2")
        kb.op("act", lambda e: e.activation(out=rstd.t[:], in_=ss.t[:], func=AF.Ln, scale=1.0 / DM, bias=EPS), reads=[ss], writes=[rstd])
        kb.op("act", lambda e: e.activation(out=rstd.t[:], in_=rstd.t[:], func=AF.Exp, scale=-0.5), reads=[rstd], writes=[rstd])
        if router is not None:
            nf = router["nf"]
            kb.op("dve", lambda e: e.scalar_tensor_tensor(out=nf.t[:], in0=srcap, scalar=rstd.t[:, 0:1], in1=gB.t[:],
                                                           op0=ALU.mult, op1=ALU.mult),
                  reads=[srcbuf, rstd, gB], writes=[nf])
            kb.op("act", lambda e: e.copy(out=nb.t[:], in_=nf.t[:]), reads=[nf], writes=[nb])
        else:
            kb.op("dve", lambda e: e.scalar_tensor_tensor(out=nb.t[:], in0=srcap, scalar=rstd.t[:, 0:1], in1=gB.t[:],
                                                           op0=ALU.mult, op1=ALU.mult),
                  reads=[srcbuf, rstd, gB], writes=[nb])
        ck("n3")
        for k0 in range(0, KC, 4):
            n4 = min(4, KC - k0)
            pT = rotp.next()
            for j in range(n4):
                kb.op("pe", lambda e, j=j: e.transpose(out=pT.t[:, j, :], in_=nb.t[:, (k0 + j) * 128:(k0 + j + 1) * 128],
                                                       identity=cm(C_ID)),
                      reads=[nb, cst], writes=[pT], sig=(j == n4 - 1))
            kb.op("act", lambda e: e.copy(out=nT.t[:, k0:k0 + n4, i * 128:(i + 1) * 128], in_=pT.t[:, 0:n4, :]),
                  reads=[pT], writes=[nT])

    def load_gvec(st, src_row, n, name):
        g = kb.sb(st, [128, n], F32, name)
        kb.dma("sp", g.t[:], src_row.partition_broadcast(128), writes=[g])
        return g

    dbg0 = dbg

    def ck(name):
        if dbg == name:
            raise _Stop()

    try:
      for l in range(depth):
        h_src = x if l == 0 else h_d
        with ExitStack() as st:
            nT = kb.sb(st, [128, KC, T], BF16, "nT")
            tab = kb.sb(st, [128, 6, T], F32, "tab")
            kb.dma("sp", tab.t[:], tab_d, writes=[tab])
            gmix = load_gvec(st, norm_mix[l:l + 1, :], DM, "gmix")
            gq = load_gvec(st, mla_q_norm[l:l + 1, :], 384, "gq")
            gkv = load_gvec(st, mla_kv_norm[l:l + 1, :], 256, "gkv")
            cqT = kb.sb(st, [128, 3, T], BF16, "cqT")
            ckvT = kb.sb(st, [128, 2, T], BF16, "ckvT")
            rotT = Rot([kb.ps(st, [128, 4, 128], BF16, "pT")])
            with ExitStack() as st2:
                hb = Rot([kb.sb(st2, [128, DM], F32, "hb") for _ in range(2)])
                tmp = (kb.sb(st2, [128, DM], F32, "junk"), kb.sb(st2, [128, 1], F32, "ss"),
                       kb.sb(st2, [128, 1], F32, "rstd"), kb.sb(st2, [128, DM], BF16, "nb"))
                ck("n0")
                for i in range(NB):
                    b = hb.next()
                    kb.dma("sp", b.t[:], h_src[i * 128:(i + 1) * 128, :], reads=[D_h], writes=[b])
                    ck("n1")
                    rms_to_T(st2, (b, b.t[:]), gmix, nT, i, rotT, tmp)
                    ck("n5")
            ck("n")
            wt = Rot([kb.sb(st, [128, KC, 512], BF16, "wt") for _ in range(3)])
            ps1 = Rot([kb.ps(st, [128, 512], F32, "ps1") for _ in range(2)])
            ps2 = Rot([kb.ps(st, [128, 512], F32, "ps2") for _ in range(2)])
            xb = Rot([kb.sb(st, [128, 512], BF16, "xb") for _ in range(2)])
            t1 = Rot([kb.sb(st, [128, 512], F32, "t1") for _ in range(2)])
            t2 = Rot([kb.sb(st, [128, 512], F32, "t2") for _ in range(2)])
            ob = Rot([kb.sb(st, [128, 512], BF16, "ob") for _ in range(3)])
            sm = (kb.sb(st, [128, 512], F32, "junk2"), kb.sb(st, [128, 1], F32, "ss2"), kb.sb(st, [128, 1], F32, "rstd2"))

            def load_w(c0, n):
                w = wt.next()
                kb.dma("pool", w.t[:, :, 0:n], w_in[l, :, c0:c0 + n].rearrange("(k p) c -> p k c", p=128), writes=[w])
                return w

            def rope_out(p1, m, perm_idx, ci, si, th, scale, dst, Dbuf):
                tk = slice(th * 512, (th + 1) * 512)
                o = ob.next()
                if perm_idx is None:
                    kb.op("act", lambda e: e.activation(out=o.t[0:m, :], in_=p1.t[0:m, :], func=AF.Copy, scale=scale),
                          reads=[p1], writes=[o])
                else:
                    xx, p2, a1, a2 = xb.next(), ps2.next(), t1.next(), t2.next()
                    kb.op("act", lambda e: e.copy(out=xx.t[0:m, :], in_=p1.t[0:m, :]), reads=[p1], writes=[xx])
                    ck("f3")
                    kb.op("pe", lambda e: e.matmul(p2.t[0:m, :], cm(perm_idx, m), xx.t[0:m, :], start=True, stop=True),
                          reads=[xx, cst], writes=[p2])
                    ck("f4")
                    kb.op("dve", lambda e: e.tensor_tensor(out=a1.t[0:m, :], in0=p1.t[0:m, :], in1=tab.t[0:m, ci, tk],
                                                           op=ALU.mult), reads=[p1, tab], writes=[a1])
                    ck("f5a")
                    kb.op("dve", lambda e: e.tensor_tensor(out=a2.t[0:m, :], in0=p2.t[0:m, :], in1=tab.t[0:m, si, tk],
                                                           op=ALU.mult), reads=[p2, tab], writes=[a2])
                    ck("f5b")
                    kb.op("dve", lambda e: e.tensor_tensor(out=o.t[0:m, :], in0=a1.t[0:m, :], in1=a2.t[0:m, :],
                                                           op=ALU.add), reads=[a1, a2], writes=[o])
                    ck("f5")
                for df in dst:
                    kb.dma("sp", df(m, tk), o.t[0:m, :], reads=[o], writes=[Dbuf])

            def formB(c0, ncols, rope, scale, dst_ap, Dbuf, r0):
                for g0 in range(0, ncols, 512):
                    gn = min(512, ncols - g0)
                    w = load_w(c0 + g0, gn)
                    ck("f1")
                    for cc in range(0, gn, 128):
                        m = min(128, gn - cc)
                        for th in range(2):
                            p1 = ps1.next()
                            for k in range(KC):
                                kb.op("pe", lambda e, k=k: e.matmul(p1.t[0:m, :], w.t[:, k, cc:cc + m],
                                                                    nT.t[:, k, th * 512:(th + 1) * 512],
                                                                    start=(k == 0), stop=(k == KC - 1)),
                                      reads=[w, nT], writes=[p1], sig=(k == KC - 1))
                            ck("f2")
                            if rope == "64":
                                rope_out(p1, m, C_P64, 0, 1, th, scale, [dst_ap(r0 + g0 + cc)], Dbuf)
                            elif rope == "R":
                                rope_out(p1, m, C_PR, 4, 5, th, scale, [dst_ap(KD + 96 * hh + 64) for hh in range(8)], Dbuf)
                            else:
                                rope_out(p1, m, None, 0, 0, th, scale, [dst_ap(r0 + g0 + cc)], Dbuf)

            def formA(c0, ncols, post):
                for g0 in range(0, ncols, 512):
                    gn = min(512, ncols - g0)
                    w = load_w(c0 + g0, gn)
                    for i in range(NB):
                        p1 = ps1.next()
                        for k in range(KC):
                            kb.op("pe", lambda e, k=k: e.matmul(p1.t[:, 0:gn], nT.t[:, k, i * 128:(i + 1) * 128],
                                                                w.t[:, k, 0:gn], start=(k == 0), stop=(k == KC - 1)),
                                  reads=[w, nT], writes=[p1], sig=(k == KC - 1))
                        post(p1, g0, gn, i)

            def post_copy(dst_ap, Dbuf, col0, func):
                def f(p1, g0, gn, i):
                    o = ob.next()
                    kb.op("act", lambda e: e.activation(out=o.t[:, 0:gn], in_=p1.t[:, 0:gn], func=func),
                          reads=[p1], writes=[o])
                    kb.dma("sp", dst_ap(i)[:, col0 + g0:col0 + g0 + gn], o.t[:, 0:gn],
                           reads=[o], writes=[Dbuf])
                return f

            def post_mla(gvec, n, dstT):
                def f(p1, g0, gn, i):
                    junk, ss, rstd = sm
                    o = ob.next()
                    kb.op("dve", lambda e: e.memset(ss.t[:], 0.0), writes=[ss])
                    kb.op("act", lambda e: e.activation(out=junk.t[:, 0:n], in_=p1.t[:, 0:n], func=AF.Square,
                                                        accum_out=ss.t[:]), reads=[p1], writes=[junk, ss])
                    kb.op("act", lambda e: e.activation(out=rstd.t[:], in_=ss.t[:], func=AF.Ln, scale=1.0 / n, bias=EPS), reads=[ss], writes=[rstd])
                    kb.op("act", lambda e: e.activation(out=rstd.t[:], in_=rstd.t[:], func=AF.Exp, scale=-0.5), reads=[rstd], writes=[rstd])
                    kb.op("dve", lambda e: e.scalar_tensor_tensor(out=o.t[:, 0:n], in0=p1.t[:, 0:n], scalar=rstd.t[:, 0:1],
                                                                   in1=gvec.t[:, 0:n], op0=ALU.mult, op1=ALU.mult),
                          reads=[p1, rstd, gvec], writes=[o])
                    nch = n // 128
                    pT = rotT.next()
                    for j in range(nch):
                        kb.op("pe", lambda e, j=j: e.transpose(out=pT.t[:, j, :], in_=o.t[:, j * 128:(j + 1) * 128],
                                                               identity=cm(C_ID)),
                              reads=[o, cst], writes=[pT], sig=(j == nch - 1))
                    kb.op("act", lambda e: e.copy(out=dstT.t[:, 0:nch, i * 128:(i + 1) * 128], in_=pT.t[:, 0:nch, :]),
                          reads=[pT], writes=[dstT])
                return f

            kb.barrier()
            formB(W_AQ, 1536, "64", 1.0, qdst, D_q, QA)
            ck("pB")
            formB(W_AK, 1536, "64", 1.0, kdst, D_kin, KA)
            formB(W_BQ, 512, "64", 1.0, qdst, D_q, QB)
            formB(W_BK, 128, "64", 1.0, kdst, D_kin, KB_)
            formB(W_CQ, 512, None, 0.125, qdst, D_q, QC)
            formB(W_CK, 512, None, 1.0, kdst, D_kin, KC_)
            formB(W_DKR, 32, "R", 1.0, kdst, D_kin, 0)
            ck("pB2")
            formA(W_AV, 1536, post_copy(vin_blk, D_vin, VA, AF.Copy))
            formA(W_BV, 128, post_copy(vin_blk, D_vin, VB, AF.Copy))
            formA(W_CV, 512, post_copy(vin_blk, D_vin, VC, AF.Copy))
            ck("pA")
            formA(W_DCQ, 384, post_mla(gq, 384, cqT))
            formA(W_DCKV, 256, post_mla(gkv, 256, ckvT))
            formA(W_G, 4 * DM, post_copy(gd_blk, D_g, 0, AF.Sigmoid))

            ck("pM")
            wuq = kb.sb(st, [128, 3, 768], BF16, "wuq")
            wuk = kb.sb(st, [128, 2, 1024], BF16, "wuk")
            wuv = kb.sb(st, [128, 2, 8, 64], BF16, "wuv")
            kb.dma("pool", wuq.t[:], mla_w_uq[l].rearrange("(k p) c -> p k c", p=128), writes=[wuq])
            kb.dma("pool", wuk.t[:], mla_w_ukv[l].rearrange("(k p) c -> p k c", p=128), writes=[wuk])
            for k in range(2):
                kb.dma("pool", wuv.t[:, k, :, :],
                       mla_w_ukv[l, k * 128:(k + 1) * 128, :].rearrange("p (h x) -> p h x", x=128)[:, :, 64:128],
                       writes=[wuv])
            for hh in range(8):
                for th in range(2):
                    tk = slice(th * 512, (th + 1) * 512)
                    p1 = ps1.next()
                    for k in range(3):
                        kb.op("pe", lambda e, k=k: e.matmul(p1.t[0:96, :], wuq.t[:, k, hh * 96:(hh + 1) * 96], cqT.t[:, k, tk],
                                                            start=(k == 0), stop=(k == 2)),
                              reads=[wuq, cqT], writes=[p1], sig=(k == 2))
                    rope_out(p1, 96, C_PD, 2, 3, th, 1.0, [qdst(QD + 96 * hh)], D_q)
                    p1 = ps1.next()
                    for k in range(2):
                        kb.op("pe", lambda e, k=k: e.matmul(p1.t[0:64, :], wuk.t[:, k, hh * 128:hh * 128 + 64], ckvT.t[:, k, tk],
                                                            start=(k == 0), stop=(k == 1)),
                              reads=[wuk, ckvT], writes=[p1], sig=(k == 1))
                    rope_out(p1, 64, None, 0, 0, th, 1.0, [kdst(KD + 96 * hh)], D_kin)
            for i in range(NB):
                p1 = ps1.next()
                for k in range(2):
                    kb.op("pe", lambda e, k=k: e.matmul(p1.t[:, :], ckvT.t[:, k, i * 128:(i + 1) * 128],
                                                        wuv.t[:, k, :, :].rearrange("p h d -> p (h d)"),
                                                        start=(k == 0), stop=(k == 1)),
                          reads=[wuv, ckvT], writes=[p1], sig=(k == 1))
                post_copy(vin_blk, D_vin, VD, AF.Copy)(p1, 0, 512, i)
            kb.barrier()
        ck("p1x")
        if dbg == "p1" and l == 0:
            o = nc.dram_tensor("d_q0", [QROWS, T], BF16, kind="ExternalOutput").ap()
            for r0 in range(0, QROWS, 128):
                kb.dma("sp", o[r0:r0 + 128, :], qT_d[r0:r0 + 128, :], reads=[D_q], writes=[D_h])
        for j in range(len(K_CH)):
            kb.allgather(kin_c[j], kout_c[j], reads=[D_kin], writes=[D_kout])
        for j in range(len(V_CH)):
            kb.allgather(vin_c[j], vout_c[j], reads=[D_vin], writes=[D_vout])
        if dbg == "p1" and l == 0:
            break

        with ExitStack() as st23:
            with ExitStack() as st:
                obr = kb.sb(st, [128, NB, 2048], BF16, "obr")
                sexp = kb.sb(st, [128, 8], F32, "sexp")
                kb.dma("sp", sexp.t[:], sinks[l:l + 1, :].partition_broadcast(128), writes=[sexp])
                kb.op("act", lambda e: e.activation(out=sexp.t[:], in_=sexp.t[:], func=AF.Exp), reads=[sexp], writes=[sexp])
                pss = Rot([kb.ps(st, [128, 512], F32, "pss") for _ in range(2)])
                psb = Rot([kb.ps(st, [128, 512], F32, "psb") for _ in range(2)])
                psO = Rot([kb.ps(st, [128, NB, 128], F32, "psO") for _ in range(2)])
                Pt = Rot([kb.sb(st, [128, 512], BF16, "Pt") for _ in range(3)])
                Et = Rot([kb.sb(st, [128, 512], F32, "Et") for _ in range(2)])
                Ut = Rot([kb.sb(st, [128, 512], BF16, "Ut") for _ in range(2)])
                Rt = kb.sb(st, [128, 512], BF16, "Rt")
                den = Rot([kb.sb(st, [128, NB, 1], F32, "den") for _ in range(2)])

                def load_mixer(stm, rows, nck, krow0, nh_v, vcol0, ncq, qrow0, bq=False):
                    KT = kb.sb(stm, [128, nck, 16, 128], BF16, "KT")
                    for c in range(nck):
                        for r in range(2):
                            src = kout_rows(r, krow0 + c * rows, rows)
                            kb.dma("sp", KT.t[0:rows, c, :, :].rearrange("p (i r) t -> p i r t", r=2)[:, :, r, :],
                                   src.rearrange("p (i t) -> p i t", t=128), reads=[D_kout], writes=[KT])
                    V = kb.sb(stm, [128, 16, nh_v, 65], BF16, "V")
                    kb.op("pool", lambda e: e.memset(V.t[:, :, :, 64:65], 1.0), writes=[V])
                    for g in range(16):
                        r, i = g % 2, g // 2
                        kb.dma("sp", V.t[:, g, :, 0:64],
                               vout_blk(r, i)[:, vcol0:vcol0 + nh_v * 64].rearrange(
                                   "t (h d) -> t h d", d=64), reads=[D_vout], writes=[V])
                    QT = kb.sb(stm, [128, ncq, T], BF16, "QT")
                    if bq:
                        for h in range(8):
                            kvh = h // 4
                            kb.dma("sp", QT.t[kvh * 64:(kvh + 1) * 64, h % 4, :], qT_d[qrow0 + 64 * h: qrow0 + 64 * (h + 1), :],
                                   reads=[D_q], writes=[QT])
                    else:
                        kb.dma("sp", QT.t[0:rows, :, :],
                               qT_d[qrow0:qrow0 + ncq * rows, :].rearrange("(c p) n -> p c n", p=rows),
                               reads=[D_q], writes=[QT])
                    return KT, V, QT

                def head_ap(Tt, dk, hidx):
                    if dk == 96:
                        return hidx, slice(0, 96)
                    return hidx // 2, slice((hidx % 2) * 64, (hidx % 2) * 64 + 64)

                def full_units(kind):
                    us = []
                    for kbk in range(16):
                        ilo = kbk // 2
                        blocks = list(range(ilo, NB))
                        j = kbk - 2 * ilo
                        for s0 in range(0, len(blocks), 4):
                            bl = blocks[s0:s0 + 4]
                            masks = {}
                            if kind == "A2":
                                for i in bl:
                                    masks[i] = MIDX["A2f"]
                            if ilo in bl:
                                masks[ilo] = MIDX["%s%d" % (kind, j)]
                            us.append((kbk, bl, masks))
                    return us

                def band_units(kind, offs):
                    us = []
                    for i in range(NB):
                        for j in offs:
                            if 2 * i + j < 0:
                                continue
                            us.append((2 * i + j, [i], {i: MIDX["%s%d" % (kind, j)]}))
                    return us

                def run_softmax_head(parts, mixer, h, sink_h=None):
                    O = psO.next()
                    kb.op("dve", lambda e: e.memset(O.t[:], 0.0), writes=[O])
                    total = {}
                    for p in parts:
                        for (kbk, bl, masks) in p[8]:
                            for i in bl:
                                total[i] = total.get(i, 0) + 1
                    seen = {}
                    for (KT, kch, kps, QT, qch, qps, V, vh, units, scale) in parts:
                        for (kbk, bl, masks) in units:
                            n = 128 * len(bl)
                            c0 = bl[0] * 128
                            s_ps = pss.next()
                            kb.op("pe", lambda e: e.matmul(s_ps.t[:, 0:n], KT.t[kps, kch, kbk, :], QT.t[qps, qch, c0:c0 + n],
                                                           start=True, stop=True), reads=[KT, QT], writes=[s_ps])
                            P = Pt.next()
                            kb.op("act", lambda e: e.activation(out=P.t[:, 0:n], in_=s_ps.t[:, 0:n], func=AF.Exp, scale=scale),
                                  reads=[s_ps], writes=[P])
                            mi = sorted(masks.items())
                            if mi and all(m == mi[0][1] for _, m in mi) and len(mi) == len(bl) and len(bl) > 1:
                                midx = mi[0][1]
                                kb.op("dve", lambda e: e.tensor_tensor(
                                    out=P.t[:, 0:n].rearrange("p (b q) -> p b q", q=128),
                                    in0=P.t[:, 0:n].rearrange("p (b q) -> p b q", q=128),
                                    in1=cst.t[:, midx:midx + 1, :].broadcast_to([128, len(bl), 128]), op=ALU.mult),
                                    reads=[P, cst], writes=[P])
                            else:
                                for (i, midx) in mi:
                                    o0 = (i - bl[0]) * 128
                                    kb.op("dve", lambda e, o0=o0, midx=midx: e.tensor_tensor(
                                        out=P.t[:, o0:o0 + 128], in0=P.t[:, o0:o0 + 128], in1=cst.t[:, midx, :], op=ALU.mult),
                                        reads=[P, cst], writes=[P])
                            for i in bl:
                                o0 = (i - bl[0]) * 128
                                seen[i] = seen.get(i, 0) + 1
                                kb.op("pe", lambda e, i=i, o0=o0: e.matmul(O.t[:, i, 0:65], P.t[:, o0:o0 + 128], V.t[:, kbk, vh, :],
                                                                          start=False, stop=(seen[i] == total[i]), skip_group_check=True),
                                      reads=[P, V], writes=[O])
                    dn = den.next()
                    if sink_h is not None:
                        kb.op("dve", lambda e: e.tensor_scalar(out=dn.t[:], in0=O.t[:, :, 64:65], scalar1=sexp.t[:, sink_h:sink_h + 1],
                                                                scalar2=None, op0=ALU.add), reads=[O, sexp], writes=[dn])
                        kb.op("dve", lambda e: e.reciprocal(out=dn.t[:], in_=dn.t[:]), reads=[dn], writes=[dn])
                    else:
                        kb.op("dve", lambda e: e.reciprocal(out=dn.t[:], in_=O.t[:, :, 64:65]), reads=[O], writes=[dn])
                    kb.op("dve", lambda e: e.tensor_tensor(out=obr.t[:, :, mixer * 512 + h * 64: mixer * 512 + h * 64 + 64],
                                                           in0=O.t[:, :, 0:64], in1=dn.t[:].broadcast_to([128, NB, 64]),
                                                           op=ALU.mult), reads=[O, dn], writes=[obr])

                with ExitStack() as stm:
                    KT, V, QT = load_mixer(stm, 128, 12, KA, 24, VA, 12, QA)
                    uA = [band_units("A0", (-1, 0, 1)), band_units("A1", (-4, -3, -2, -1, 0, 1)), full_units("A2")]
                    for h in range(8):
                        parts = []
                        for g in range(3):
                            hh = g * 8 + h
                            ch, psl = head_ap(KT, 64, hh)
                            parts.append((KT, ch, psl, QT, ch, psl, V, hh, uA[g], 0.125))
                        run_softmax_head(parts, 0, h)
                    kb.barrier()
                with ExitStack() as stm:
                    KT, V, QT = load_mixer(stm, 128, 1, KB_, 2, VB, 4, QB, bq=True)
                    uB = band_units("B", (-1, 0, 1))
                    for h in range(8):
                        kvh = h // 4
                        kps = slice(kvh * 64, kvh * 64 + 64)
                        run_softmax_head([(KT, 0, kps, QT, h % 4, kps, V, kvh, uB, 0.125)], 1, h, sink_h=h)
                    kb.barrier()
                with ExitStack() as stm:
                    KT, V, QT = load_mixer(stm, 96, 8, KD, 8, VD, 8, QD)
                    uD = full_units("D")
                    for h in range(8):
                        run_softmax_head([(KT, h, slice(0, 96), QT, h, slice(0, 96), V, h, uD, 96 ** -0.5)], 3, h)
                    kb.barrier()
                with ExitStack() as stm:
                    KT, V, QT = load_mixer(stm, 128, 4, KC_, 8, VC, 4, QC)
                    for h in range(8):
                        ch, psl = head_ap(KT, 64, h)
                        for i0 in (0, 4):
                            O = psO.next()
                            kb.op("dve", lambda e: e.memset(O.t[:], 0.0), writes=[O])
                            first = True
                            seen = {}
                            kmax = 2 * (i0 + 3) + 1
                            for kbk in range(kmax, -1, -1):
                                ilo = max(kbk // 2, i0)
                                bl = list(range(ilo, i0 + 4))
                                n = 128 * len(bl)
                                c0 = ilo * 128
                                l0 = (ilo - i0) * 128
                                bmask = MIDX["C%d" % (kbk - 2 * (kbk // 2))] if kbk // 2 >= i0 else None
                                pa = pss.next()
                                kb.op("pe", lambda e: e.matmul(pa.t[:, 0:n], KT.t[psl, ch, kbk, :], QT.t[psl, ch, c0:c0 + n],
                                                               start=True, stop=True), reads=[KT, QT], writes=[pa])
                                E = Et.next()
                                U = Ut.next()
                                kb.op("act", lambda e: e.activation(out=E.t[:, 0:n], in_=pa.t[:, 0:n], func=AF.Exp),
                                      reads=[pa], writes=[E])
                                kb.op("act", lambda e: e.activation(out=U.t[:, 0:n], in_=E.t[:, 0:n], func=AF.Ln, bias=1.0),
                                      reads=[E], writes=[U])
                                if bmask is not None:
                                    kb.op("dve", lambda e: e.tensor_tensor(out=U.t[:, 0:128], in0=U.t[:, 0:128],
                                                                           in1=cst.t[:, bmask, :], op=ALU.mult),
                                          reads=[U, cst], writes=[U])
                                pb = psb.next()
                                kb.op("pe", lambda e: e.matmul(pb.t[:, 0:n], KT.t[psl, ch, kbk, :], QT.t[psl, ch, c0:c0 + n],
                                                               start=True, stop=False), reads=[KT, QT], writes=[pb], sig=False)
                                kb.op("pe", lambda e: e.matmul(pb.t[:, 0:n], cm(C_TRI), U.t[:, 0:n], start=False, stop=first),
                                      reads=[cst, U], writes=[pb], sig=first)
                                if not first:
                                    kb.op("pe", lambda e: e.matmul(pb.t[:, 0:n], cm(C_ONES), Rt.t[:, l0:l0 + n], start=False, stop=True),
                                          reads=[cst, Rt], writes=[pb])
                                P = Pt.next()
                                kb.op("act", lambda e: e.activation(out=P.t[:, 0:n], in_=pb.t[:, 0:n], func=AF.Exp),
                                      reads=[pb], writes=[P])
                                if bmask is not None:
                                    kb.op("dve", lambda e: e.tensor_tensor(out=P.t[:, 0:128], in0=P.t[:, 0:128],
                                                                           in1=cst.t[:, bmask, :], op=ALU.mult),
                                          reads=[P, cst], writes=[P])
                                for i in bl:
                                    o0 = (i - ilo) * 128
                                    nvis = 2 * i + 2
                                    seen[i] = seen.get(i, 0) + 1
                                    kb.op("pe", lambda e, i=i, o0=o0: e.matmul(O.t[:, i - i0, 0:64], P.t[:, o0:o0 + 128], V.t[:, kbk, h, 0:64],
                                                                              start=False, stop=(seen[i] == nvis), skip_group_check=True),
                                          reads=[P, V], writes=[O])
                                if kbk > 0:
                                    if first:
                                        kb.op("dve", lambda e: e.memset(Rt.t[:], 0.0), writes=[Rt])
                                    kb.op("dve", lambda e: e.tensor_tensor(out=Rt.t[:, l0:l0 + n], in0=Rt.t[:, l0:l0 + n],
                                                                           in1=U.t[:, 0:n], op=ALU.add),
                                          reads=[Rt, U], writes=[Rt])
                                first = False
                            kb.op("act", lambda e: e.copy(out=obr.t[:, i0:i0 + 4, 2 * 512 + h * 64: 2 * 512 + h * 64 + 64],
                                                          in_=O.t[:, 0:4, 0:64]), reads=[O], writes=[obr])
                    kb.barrier()
                for i in range(NB):
                    kb.dma("sp", obr_d[i * 128:(i + 1) * 128, :], obr.t[:, i, :], reads=[obr], writes=[D_obr])
                kb.barrier()

            with ExitStack() as st3:
                with ExitStack() as st:
                    wb = kb.sb(st, [128, 16, DM], BF16, "wb")
                    kb.dma("pool", wb.t[:], w_branch[l].rearrange("(c p) d -> p c d", p=128), writes=[wb])
                    oT = Rot([kb.sb(st, [128, 16, 128], BF16, "oT") for _ in range(2)])
                    obk = Rot([kb.sb(st, [128, 2048], BF16, "obk") for _ in range(2)])
                    mTb = Rot([kb.sb(st, [128, KC, 128], BF16, "mTb") for _ in range(2)])
                    gt = Rot([kb.sb(st, [128, 4 * DM], BF16, "gt") for _ in range(2)])
                    mg = Rot([kb.sb(st, [128, DM], F32, "mg") for _ in range(2)])
                    mb = Rot([kb.sb(st, [128, DM], BF16, "mb") for _ in range(2)])
                    tm = Rot([kb.sb(st, [128, CG], F32, "tm") for _ in range(2)])
                    rotT = Rot([kb.ps(st, [128, 4, 128], BF16, "pT") for _ in range(2)])
                    psm = Rot([kb.ps(st, [128, CG], F32, "psm") for _ in range(3)])
                    for i in range(NB):
                        o_t, g_t, m_g, m_b = oT.next(), gt.next(), mg.next(), mb.next()
                        ob_, mT_ = obk.next(), mTb.next()
                        kb.dma("sp", ob_.t[:], obr_d[i * 128:(i + 1) * 128, :], reads=[D_obr], writes=[ob_])
                        kb.dma("sp", g_t.t[:], g_d[i * 128:(i + 1) * 128, :], reads=[D_g], writes=[g_t])
                        for c0 in range(0, 16, 4):
                            pT = rotT.next()
                            for j in range(4):
                                kb.op("pe", lambda e, j=j: e.transpose(out=pT.t[:, j, :], in_=ob_.t[:, (c0 + j) * 128:(c0 + j + 1) * 128],
                                                                       identity=cm(C_ID)), reads=[ob_, cst], writes=[pT], sig=(j == 3))
                            kb.op("act", lambda e: e.copy(out=o_t.t[:, c0:c0 + 4, :], in_=pT.t[:]), reads=[pT], writes=[o_t])
                        for n_ in range(4):
                            for cg in range(NCG):
                                pm = psm.next()
                                for c in range(4):
                                    kb.op("pe", lambda e, c=c: e.matmul(pm.t[:], o_t.t[:, 4 * n_ + c, :], wb.t[:, 4 * n_ + c, cg * CG:(cg + 1) * CG],
                                                                        start=(c == 0), stop=(c == 3)),
                                          reads=[o_t, wb], writes=[pm], sig=(c == 3))
                                gsl = g_t.t[:, n_ * DM + cg * CG: n_ * DM + (cg + 1) * CG]
                                if n_ == 0:
                                    kb.op("dve", lambda e: e.tensor_tensor(out=m_g.t[:, cg * CG:(cg + 1) * CG], in0=pm.t[:], in1=gsl,
                                                                           op=ALU.mult), reads=[pm, g_t], writes=[m_g])
                                else:
                                    tt = tm.next()
                                    kb.op("dve", lambda e: e.tensor_tensor(out=tt.t[:], in0=pm.t[:], in1=gsl, op=ALU.mult),
                                          reads=[pm, g_t], writes=[tt])
                                    kb.op("pool", lambda e: e.tensor_tensor(out=m_g.t[:, cg * CG:(cg + 1) * CG],
                                                                            in0=m_g.t[:, cg * CG:(cg + 1) * CG], in1=tt.t[:], op=ALU.add),
                                          reads=[m_g, tt], writes=[m_g])
                        kb.op("act", lambda e: e.copy(out=m_b.t[:], in_=m_g.t[:]), reads=[m_g], writes=[m_b])
                        for k0 in range(0, KC, 4):
                            n4 = min(4, KC - k0)
                            pT = rotT.next()
                            for j in range(n4):
                                kb.op("pe", lambda e, j=j: e.transpose(out=pT.t[:, j, :], in_=m_b.t[:, (k0 + j) * 128:(k0 + j + 1) * 128],
                                                                       identity=cm(C_ID)), reads=[m_b, cst], writes=[pT], sig=(j == n4 - 1))
                            kb.op("act", lambda e: e.copy(out=mT_.t[:, k0:k0 + n4, :], in_=pT.t[:, 0:n4, :]),
                                  reads=[pT], writes=[mT_])
                        kb.dma("sp", mT_d.rearrange("(k p) n -> p k n", p=128)[:, :, i * 128:(i + 1) * 128], mT_.t[:],
                               reads=[mT_], writes=[D_mT])
                    kb.barrier()
                moe = (l % 2 == 1)
                st4 = ExitStack()
                acc = kb.sb(st4, [128, NB, DM], F32, "acc")
                n2T = kb.sb(st4, [128, KC, T], BF16, "n2T")
                cw = kb.sb(st4, [128, NB, NE], F32, "cw")
                with ExitStack() as st:
                    wo = Rot([kb.sb(st, [128, KC, CG], BF16, "wo") for _ in range(2)])
                    mT = kb.sb(st, [128, KC, T], BF16, "mT")
                    kb.dma("sp", mT.t[:], mT_d.rearrange("(k p) n -> p k n", p=128), reads=[D_mT], writes=[mT])
                    hcg = Rot([kb.sb(st, [128, CG], F32, "hcg") for _ in range(3)])
                    pso = Rot([kb.ps(st, [128, CG], F32, "pso") for _ in range(2)])
                    for cg in range(NCG):
                        w = wo.next()
                        kb.dma("pool", w.t[:], w_out[l, :, cg * CG:(cg + 1) * CG].rearrange("(k p) c -> p k c", p=128), writes=[w])
                        for i in range(NB):
                            hh_ = hcg.next()
                            kb.dma("sp", hh_.t[:], h_src[i * 128:(i + 1) * 128, cg * CG:(cg + 1) * CG], reads=[D_h], writes=[hh_])
                            po = pso.next()
                            for k in range(KC):
                                kb.op("pe", lambda e, k=k: e.matmul(po.t[:], mT.t[:, k, i * 128:(i + 1) * 128], w.t[:, k, :],
                                                                    start=(k == 0), stop=(k == KC - 1)),
                                      reads=[mT, w], writes=[po], sig=(k == KC - 1))
                            kb.op("dve", lambda e: e.tensor_tensor(out=acc.t[:, i, cg * CG:(cg + 1) * CG], in0=po.t[:], in1=hh_.t[:],
                                                                   op=ALU.add), reads=[po, hh_], writes=[acc])
                    kb.barrier()
                with ExitStack() as st:
                    gffn = load_gvec(st, norm_ffn[l:l + 1, :], DM, "gffn")
                    rotT = Rot([kb.ps(st, [128, 4, 128], BF16, "pT") for _ in range(2)])
                    tmp = (kb.sb(st, [128, DM], F32, "junk"), kb.sb(st, [128, 1], F32, "ss"),
                           kb.sb(st, [128, 1], F32, "rstd"), kb.sb(st, [128, DM], BF16, "nb"))
                    router = None
                    if moe:
                        router = dict(nf=kb.sb(st, [128, DM], F32, "nf"))
                        wr = kb.sb(st, [128, KC, NE], F32, "wr")
                        kb.dma("sp", wr.t[:], router_w[0].rearrange("(k p) e -> p k e", p=128), writes=[wr])
                        pTf = Rot([kb.ps(st, [128, 128], F32, "pTf") for _ in range(2)])
                        nfT = Rot([kb.sb(st, [128, 128], F32, "nfT") for _ in range(2)])
                        psl_ = kb.ps(st, [128, NE], F32, "psl")
                        lg = kb.sb(st, [128, NE], F32, "lg")
                        l2 = kb.sb(st, [128, NE], F32, "l2")
                        mk1 = kb.sb(st, [128, NE], F32, "mk1")
                        mk2 = kb.sb(st, [128, NE], F32, "mk2")
                        m1 = kb.sb(st, [128, 1], F32, "m1")
                        m2 = kb.sb(st, [128, 1], F32, "m2")
                        ee = kb.sb(st, [128, 1], F32, "ee")
                        w1 = kb.sb(st, [128, 1], F32, "w1")
                        w2 = kb.sb(st, [128, 1], F32, "w2")
                    for i in range(NB):
                        rms_to_T(st, (acc, acc.t[:, i, :]), gffn, n2T, i, rotT, tmp, router)
                        if moe:
                            nf = router["nf"]
                            for k in range(KC):
                                pf, nt_ = pTf.next(), nfT.next()
                                kb.op("pe", lambda e: e.transpose(out=pf.t[:], in_=nf.t[:, k * 128:(k + 1) * 128], identity=identf.t[:]),
                                      reads=[nf, identf], writes=[pf])
                                kb.op("dve", lambda e: e.tensor_copy(out=nt_.t[:], in_=pf.t[:]), reads=[pf], writes=[nt_])
                                kb.op("pe", lambda e: e.matmul(psl_.t[:], nt_.t[:], wr.t[:, k, :], start=(k == 0), stop=(k == KC - 1)),
                                      reads=[nt_, wr], writes=[psl_])
                            kb.op("dve", lambda e: e.tensor_copy(out=lg.t[:], in_=psl_.t[:]), reads=[psl_], writes=[lg])
                            kb.op("dve", lambda e: e.reduce_max(out=m1.t[:], in_=lg.t[:], axis=mybir.AxisListType.X), reads=[lg], writes=[m1])
                            kb.op("dve", lambda e: e.tensor_scalar(out=mk1.t[:], in0=lg.t[:], scalar1=m1.t[:, 0:1], scalar2=None,
                                                                    op0=ALU.is_ge), reads=[lg, m1], writes=[mk1])
                            kb.op("dve", lambda e: e.scalar_tensor_tensor(out=l2.t[:], in0=mk1.t[:], scalar=-1e30, in1=lg.t[:],
                                                                           op0=ALU.mult, op1=ALU.add), reads=[mk1, lg], writes=[l2])
                            kb.op("dve", lambda e: e.reduce_max(out=m2.t[:], in_=l2.t[:], axis=mybir.AxisListType.X), reads=[l2], writes=[m2])
                            kb.op("dve", lambda e: e.tensor_scalar(out=mk2.t[:], in0=l2.t[:], scalar1=m2.t[:, 0:1], scalar2=None,
                                                                    op0=ALU.is_ge), reads=[l2, m2], writes=[mk2])
                            kb.op("dve", lambda e: e.tensor_tensor(out=ee.t[:], in0=m2.t[:], in1=m1.t[:], op=ALU.subtract),
                                  reads=[m1, m2], writes=[ee])
                            kb.op("act", lambda e: e.activation(out=ee.t[:], in_=ee.t[:], func=AF.Exp), reads=[ee], writes=[ee])
                            kb.op("dve", lambda e: e.tensor_scalar(out=w1.t[:], in0=ee.t[:], scalar1=1.0, scalar2=None, op0=ALU.add),
                                  reads=[ee], writes=[w1])
                            kb.op("dve", lambda e: e.reciprocal(out=w1.t[:], in_=w1.t[:]), reads=[w1], writes=[w1])
                            kb.op("dve", lambda e: e.tensor_tensor(out=w2.t[:], in0=ee.t[:], in1=w1.t[:], op=ALU.mult),
                                  reads=[ee, w1], writes=[w2])
                            kb.op("dve", lambda e: e.tensor_scalar(out=mk1.t[:], in0=mk1.t[:], scalar1=w1.t[:, 0:1], scalar2=None,
                                                                    op0=ALU.mult), reads=[mk1, w1], writes=[mk1])
                            kb.op("dve", lambda e: e.scalar_tensor_tensor(out=cw.t[:, i, :], in0=mk2.t[:], scalar=w2.t[:, 0:1], in1=mk1.t[:],
                                                                           op0=ALU.mult, op1=ALU.add), reads=[mk2, w2, mk1], writes=[cw])
                    kb.barrier()
                with ExitStack() as st:
                    FG = 256
                    wg = Rot([kb.sb(st, [128, KC, FG], BF16, "wg") for _ in range(2)])
                    wu = Rot([kb.sb(st, [128, KC, FG], BF16, "wu") for _ in range(2)])
                    wd = Rot([kb.sb(st, [128, FG // 128, DM], BF16, "wd") for _ in range(2)])
                    hT = Rot([kb.sb(st, [128, FG // 128, T], BF16, "hT") for _ in range(2)])
                    sg = Rot([kb.sb(st, [128, 512], F32, "sg") for _ in range(2)])
                    psg = Rot([kb.ps(st, [128, 512], F32, "psg") for _ in range(2)])
                    psu = Rot([kb.ps(st, [128, 512], F32, "psu") for _ in range(2)])
                    psd = Rot([kb.ps(st, [128, CG], F32, "psd") for _ in range(3)])
                    if moe:
                        experts = [(moe_w_gate[e_], moe_w_up[e_], moe_w_down[e_], DFE, e_) for e_ in range(NE)]
                    else:
                        experts = [(ffn_w_gate[0], ffn_w_up[0], ffn_w_down[0], DFF, None)]
                    for (Wg, Wu, Wd, dff, eidx) in experts:
                        for f0 in range(0, dff, FG):
                            a, b, d, hT_ = wg.next(), wu.next(), wd.next(), hT.next()
                            kb.dma("pool", a.t[:], Wg[:, f0:f0 + FG].rearrange("(k p) c -> p k c", p=128), writes=[a])
                            kb.dma("pool", b.t[:], Wu[:, f0:f0 + FG].rearrange("(k p) c -> p k c", p=128), writes=[b])
                            kb.dma("pool", d.t[:], Wd[f0:f0 + FG, :].rearrange("(c p) d -> p c d", p=128), writes=[d])
                            for fc in range(FG // 128):
                                for th in range(2):
                                    tk = slice(th * 512, (th + 1) * 512)
                                    pg, pu = psg.next(), psu.next()
                                    for k in range(KC):
                                        kb.op("pe", lambda e, k=k: e.matmul(pg.t[:], a.t[:, k, fc * 128:(fc + 1) * 128], n2T.t[:, k, tk],
                                                                            start=(k == 0), stop=(k == KC - 1)),
                                              reads=[a, n2T], writes=[pg], sig=(k == KC - 1))
                                    for k in range(KC):
                                        kb.op("pe", lambda e, k=k: e.matmul(pu.t[:], b.t[:, k, fc * 128:(fc + 1) * 128], n2T.t[:, k, tk],
                                                                            start=(k == 0), stop=(k == KC - 1)),
                                              reads=[b, n2T], writes=[pu], sig=(k == KC - 1))
                                    s_ = sg.next()
                                    kb.op("act", lambda e: e.activation(out=s_.t[:], in_=pg.t[:], func=AF.Silu), reads=[pg], writes=[s_])
                                    kb.op("dve", lambda e: e.tensor_tensor(out=hT_.t[:, fc, tk], in0=pu.t[:], in1=s_.t[:], op=ALU.mult),
                                          reads=[pu, s_], writes=[hT_])
                            for i in range(NB):
                                for cg in range(NCG):
                                    pd = psd.next()
                                    nf_ = FG // 128
                                    for fc in range(nf_):
                                        kb.op("pe", lambda e, fc=fc: e.matmul(pd.t[:], hT_.t[:, fc, i * 128:(i + 1) * 128], d.t[:, fc, cg * CG:(cg + 1) * CG],
                                                                              start=(fc == 0), stop=(fc == nf_ - 1)),
                                              reads=[hT_, d], writes=[pd], sig=(fc == nf_ - 1))
                                    asl = acc.t[:, i, cg * CG:(cg + 1) * CG]
                                    if eidx is None:
                                        kb.op("dve", lambda e: e.tensor_tensor(out=asl, in0=pd.t[:], in1=asl, op=ALU.add),
                                              reads=[pd, acc], writes=[acc])
                                    else:
                                        kb.op("dve", lambda e: e.scalar_tensor_tensor(out=asl, in0=pd.t[:], scalar=cw.t[:, i, eidx:eidx + 1],
                                                                                       in1=asl, op0=ALU.mult, op1=ALU.add),
                                              reads=[pd, acc, cw], writes=[acc])
                    kb.barrier()
                with ExitStack() as st:
                    if l < depth - 1:
                        for i in range(NB):
                            kb.dma("sp", h_d[i * 128:(i + 1) * 128, :], acc.t[:, i, :], reads=[acc], writes=[D_h])
                    else:
                        gfin = load_gvec(st, norm_final[0:1, :], DM, "gfin")
                        junk = kb.sb(st, [128, DM], F32, "junk")
                        ss = kb.sb(st, [128, 1], F32, "ss")
                        rstd = kb.sb(st, [128, 1], F32, "rstd")
                        yo = Rot([kb.sb(st, [128, DM], F32, "yo") for _ in range(2)])
                        for i in range(NB):
                            kb.op("dve", lambda e: e.memset(ss.t[:], 0.0), writes=[ss])
                            kb.op("act", lambda e: e.activation(out=junk.t[:], in_=acc.t[:, i, :], func=AF.Square, accum_out=ss.t[:]),
                                  reads=[acc], writes=[junk, ss])
                            kb.op("act", lambda e: e.activation(out=rstd.t[:], in_=ss.t[:], func=AF.Ln, scale=1.0 / DM, bias=EPS), reads=[ss], writes=[rstd])
                            kb.op("act", lambda e: e.activation(out=rstd.t[:], in_=rstd.t[:], func=AF.Exp, scale=-0.5), reads=[rstd], writes=[rstd])
                            o = yo.next()
                            kb.op("dve", lambda e: e.scalar_tensor_tensor(out=o.t[:], in0=acc.t[:, i, :], scalar=rstd.t[:, 0:1], in1=gfin.t[:],
                                                                           op0=ALU.mult, op1=ALU.mult), reads=[acc, rstd, gfin], writes=[o])
                            kb.dma("sp", y[i * 128:(i + 1) * 128, :], o.t[:], reads=[o], writes=[D_h])
                    kb.barrier()
                st4.close()
                ck("l%d" % l)
    except _Stop:
        dbg = "stopped"
    if dbg0 in ("l0", "l1"):
        for nm, src, shp, dt_ in (("d_obr", obr_d, [T, 2048], BF16), ("d_h", h_d, [T, DM], F32), ("d_mT", mT_d, [DM, T], BF16)):
            o = nc.dram_tensor(nm, shp, dt_, kind="ExternalOutput").ap()
            for r0 in range(0, shp[0], 128):
                kb.dma("sp", o[r0:r0 + 128, :], src[r0:r0 + 128, :], reads=[D_obr, D_mT, D_h], writes=[D_q])
    if dbg == "p1":
        dumps = [("d_q", qT_d, [QROWS, T]), ("d_g", g_d, [T, 4 * DM])]
        dumps += [("d_k%d" % j, kout_c[j], [2 * (b_ - a_), T]) for j, (a_, b_) in enumerate(K_CH)]
        dumps += [("d_v%d" % j, vout_c[j], [2 * (b_ - a_) * 128, VCOLS]) for j, (a_, b_) in enumerate(V_CH)]
        for nm, src, shp in dumps:
            o = nc.dram_tensor(nm, shp, BF16, kind="ExternalOutput").ap()
            for r0 in range(0, shp[0], 128):
                kb.dma("sp", o[r0:r0 + 128, :], src[r0:r0 + 128, :], reads=[D_q, D_kout, D_vout, D_g], writes=[D_h])
    kb.barrier()
    if dbg != "stopped":
        top.close()
    return nc


def _masks(p):
    k = np.arange(128)[:, None]
    q = np.arange(128)[None, :]

    def m(kind, delta):
        d = 128 * delta + q - k
        if kind == "D":
            v = d >= 0
        elif kind == "C":
            v = d >= 1
        elif kind == "B":
            v = (d >= 0) & (d <= 127)
        elif kind == "A0":
            v = (d >= 0) & (d <= 128)
        elif kind == "A1":
            v = (d >= 0) & (d % 4 == 0) & (d <= 512)
        elif kind == "A2":
            v = (d >= 0) & (d % 16 == 0)
        return v.astype(np.float32)

    out = {}
    for j in (0, 1):
        out["D%d" % j] = m("D", p - j)
        out["C%d" % j] = m("C", p - j)
        out["A2%d" % j] = m("A2", p - j)
    out["A2f"] = m("A2", 3)
    for j in (-1, 0, 1):
        out["B%d" % j] = m("B", p - j)
        out["A0%d" % j] = m("A0", p - j)
    for j in (-4, -3, -2, -1, 0, 1):
        out["A1%d" % j] = m("A1", p - j)
    return out


def _consts(p):
    c = np.zeros((128, NCST, 128), np.float32)
    c[:, C_ID, :] = np.eye(128)
    for m_ in range(128):
        d = m_ % 64
        c[(m_ - d) + (d + 32) % 64, C_P64, m_] = 1.0
    for m_ in range(64, 96):
        d = m_ - 64
        c[64 + (d + 16) % 32, C_PD, m_] = 1.0
    for m_ in range(32):
        c[(m_ + 16) % 32, C_PR, m_] = 1.0
    jj = np.arange(128)[:, None]
    ss = np.arange(128)[None, :]
    c[:, C_TRI, :] = -(jj >= ss).astype(np.float32)
    c[:, C_ONES, :] = -1.0
    mk = _masks(p)
    for n in MASK_NAMES:
        c[:, MIDX[n], :] = mk[n]
    return c


def _tables(p):
    pos = (128 * (2 * np.arange(NB)[:, None] + p) + np.arange(128)[None, :]).reshape(-1).astype(np.float32)
    tab = np.zeros((128, 6, T), np.float32)
    f64 = (10000.0 ** (-2.0 * np.arange(32, dtype=np.float32) / 64)).astype(np.float32)
    f32_ = (10000.0 ** (-2.0 * np.arange(16, dtype=np.float32) / 32)).astype(np.float32)
    for r in range(128):
        d = r % 64
        ang = pos * f64[d % 32]
        tab[r, 0] = np.cos(ang)
        tab[r, 1] = np.sin(ang) * (-1.0 if d < 32 else 1.0)
    tab[0:64, 2] = 1.0
    for r in range(32):
        ang = pos * f32_[r % 16]
        sgn = -1.0 if r < 16 else 1.0
        tab[64 + r, 2] = np.cos(ang)
        tab[64 + r, 3] = np.sin(ang) * sgn
        tab[r, 4] = np.cos(ang)
        tab[r, 5] = np.sin(ang) * sgn
    return tab


_NC_CACHE = {}


def make_in_maps(inp):
    f = lambda a: np.ascontiguousarray(np.asarray(a, dtype=np.float32))
    xs = f(inp["x"])
    B, S_, DM = xs.shape
    depth = inp["w_in"].shape[0]
    shared = dict(
        w_in=f(inp["w_in"]), w_branch=f(inp["w_branch"]).reshape(depth, 4 * 512, DM), w_out=f(inp["w_out"]),
        norm_mix=f(inp["norm_mix"]), norm_ffn=f(inp["norm_ffn"]), norm_final=f(inp["norm_final"]).reshape(1, DM),
        sinks=f(inp["sinks"]), mla_q_norm=f(inp["mla_q_norm"]), mla_kv_norm=f(inp["mla_kv_norm"]),
        mla_w_uq=f(inp["mla_w_uq"]), mla_w_ukv=f(inp["mla_w_ukv"]),
        ffn_w_gate=f(inp["ffn_w_gate"]), ffn_w_up=f(inp["ffn_w_up"]), ffn_w_down=f(inp["ffn_w_down"]),
        router_w=f(inp["router_w"]), moe_w_gate=f(inp["moe_w_gate"])[0], moe_w_up=f(inp["moe_w_up"])[0],
        moe_w_down=f(inp["moe_w_down"])[0])
    maps = []
    for c in range(8):
        b, p = c // 2, c % 2
        m = dict(shared)
        m["x"] = np.ascontiguousarray(xs[b].reshape(16, 128, DM)[p::2].reshape(T, DM))
        m["cst"] = _consts(p)
        m["tab"] = _tables(p)
        maps.append(m)
    return maps


def kernel(**inp):
    DM = inp["x"].shape[2]
    DFF = inp["ffn_w_gate"].shape[2]
    DFE = inp["moe_w_gate"].shape[3]
    key = (DM, DFF, DFE)
    if key not in _NC_CACHE:
        _NC_CACHE[key] = build(DM, DFF, DFE)
    nc = _NC_CACHE[key]
    maps = make_in_maps(inp)
    res = run_bass_kernel_spmd(nc, maps, core_ids=list(range(8)))
    out = np.zeros((4, 16, 128, DM), np.float32)
    for c in range(8):
        b, p = c // 2, c % 2
        out[b, p::2] = np.asarray(res.results[c]["y"]).reshape(NB, 128, DM)
    return out.reshape(4, S, DM)
```

```python
import numpy as np
from contextlib import ExitStack
import concourse.bass as bass
import concourse.mybir as mybir
from concourse.bass_utils import run_bass_kernel_spmd

F32 = mybir.dt.float32
BF16 = mybir.dt.bfloat16
AF = mybir.ActivationFunctionType
ALU = mybir.AluOpType

S = 2048
T = 1024
NB = 8
EPS = 1e-6
NDMA = 24
COMPUTE = ("pe", "act", "dve", "pool")

QA, QB, QC, QD, QROWS = 0, 1536, 2048, 2560, 3328
KA, KB_, KC_, KD, KROWS = 0, 1536, 1664, 2176, 2944
VA, VB, VC, VD, VCOLS = 0, 1536, 1664, 2176, 2688
W_AQ, W_AK, W_AV, W_BQ, W_BK, W_BV, W_CQ, W_CK, W_CV, W_DCQ, W_DCKV, W_DKR, W_G = (
    0, 1536, 3072, 4608, 5120, 5248, 5376, 5888, 6400, 6912, 7296, 7552, 7584)

C_ID, C_P64, C_PD, C_PR, C_TRI, C_ONES = 0, 1, 2, 3, 4, 5
MASK_NAMES = (["D0", "D1", "C0", "C1", "A2f", "A20", "A21", "B-1", "B0", "B1", "A0-1", "A00", "A01"]
              + ["A1%d" % j for j in (-4, -3, -2, -1, 0, 1)])
C_MASK0 = 6
NCST = C_MASK0 + len(MASK_NAMES)
MIDX = {n: C_MASK0 + i for i, n in enumerate(MASK_NAMES)}


class Buf:
    __slots__ = ("t", "w", "r", "ps")

    def __init__(self, t, ps=False):
        self.t = t
        self.w = None
        self.r = {}
        self.ps = ps

    def __getitem__(self, k):
        return self.t[k]


class KB:
    def __init__(self, nc):
        self.nc = nc
        self.eng = dict(pe=nc.tensor, act=nc.scalar, dve=nc.vector, pool=nc.gpsimd, sp=nc.sync)
        self.sem = {e: nc.alloc_semaphore("s_" + e) for e in COMPUTE}
        self.cnt = {e: 0 for e in COMPUTE}
        self.pend = {e: False for e in COMPUTE}
        self.dsem = [nc.alloc_semaphore("s_dma%d" % i) for i in range(NDMA)]
        self.dcnt = [0] * NDMA
        self.drr = 0
        self.waited = {e: {} for e in self.eng}
        self.csem = nc.alloc_semaphore("s_cc")
        self.ccnt = 0
        self.nid = 0

    def _semh(self, key):
        if key == "cc":
            return self.csem
        return self.sem[key] if isinstance(key, str) else self.dsem[key[1]]

    def _wait(self, e, deps):
        best = {}
        for (k, v) in deps:
            if k == "pe" and e == "pe":
                continue
            if best.get(k, 0) < v:
                best[k] = v
        w = self.waited[e]
        for k, v in best.items():
            if w.get(k, 0) >= v:
                continue
            self.eng[e].wait_ge(self._semh(k), v)
            w[k] = v

    def _deps(self, reads, writes):
        deps = []
        for b in reads:
            if b.w:
                deps.append(b.w)
        for b in writes:
            if b.w:
                deps.append(b.w)
            deps.extend(b.r.items())
        return deps

    def _record(self, ev, reads, writes):
        for b in reads:
            if b.r.get(ev[0], 0) < ev[1]:
                b.r[ev[0]] = ev[1]
        for b in writes:
            b.w = ev
            b.r = {}

    def op(self, e, fn, reads=(), writes=(), sig=True):
        psr = [b for b in reads if b.ps]
        if psr:
            reads = [b for b in reads if not b.ps]
            writes = list(writes) + [b for b in psr if b not in writes]
        self._wait(e, self._deps(reads, writes))
        ins = fn(self.eng[e])
        ev = (e, self.cnt[e] + 1)
        if sig:
            ins.then_inc(self.sem[e], 1)
            self.cnt[e] += 1
            self.pend[e] = False
        else:
            self.pend[e] = True
        self._record(ev, reads, writes)

    def dma(self, q, out, in_, reads=(), writes=()):
        k = self.drr
        self.drr = (k + 1) % NDMA
        deps = self._deps(reads, writes)
        if self.dcnt[k]:
            deps.append((("d", k), self.dcnt[k]))
        self._wait(q, deps)
        ins = self.eng[q].dma_start(out=out, in_=in_)
        self.dcnt[k] += 16
        ins.then_inc(self.dsem[k], 16)
        self._record((("d", k), self.dcnt[k]), reads, writes)

    def allgather(self, in_ap, out_ap, reads=(), writes=()):
        self._wait("pool", self._deps(reads, writes))
        ins = self.nc.gpsimd.collective_compute(
            "AllGather", ALU.bypass, replica_groups=[[0, 1], [2, 3], [4, 5], [6, 7]],
            ins=[in_ap], outs=[out_ap])
        self.ccnt += 1
        ins.then_inc(self.csem, 1)
        self._record(("cc", self.ccnt), reads, writes)

    def barrier(self):
        for e in COMPUTE:
            assert not self.pend[e], e
        evs = [(e, self.cnt[e]) for e in COMPUTE if self.cnt[e]]
        evs += [(("d", k), self.dcnt[k]) for k in range(NDMA) if self.dcnt[k]]
        if self.ccnt:
            evs.append(("cc", self.ccnt))
        for e in self.eng:
            self._wait(e, [x for x in evs if x[0] != e])

    def sb(self, st, shape, dt, name=None):
        self.nid += 1
        return Buf(st.enter_context(self.nc.sbuf_tensor("%s_%d" % (name or "t", self.nid), list(shape), dt)))

    def ps(self, st, shape, dt=F32, name=None):
        self.nid += 1
        return Buf(st.enter_context(self.nc.psum_tensor("%s_%d" % (name or "p", self.nid), list(shape), dt)), ps=True)


class _Stop(Exception):
    pass


class Rot:
    def __init__(self, bufs):
        self.b = bufs
        self.i = 0

    def next(self):
        b = self.b[self.i % len(self.b)]
        self.i += 1
        return b


def build(DM, DFF, DFE, NE=8, depth=2, dbg=None):
    assert DM % 128 == 0
    KC = DM // 128
    CG = min(512, DM)
    NCG = DM // CG
    INW = W_G + 4 * DM
    nc = bass.Bass("TRN2", target_bir_lowering=False)
    kb = KB(nc)

    lite = bool(dbg) and dbg.startswith("n")
    BIGW = ("w_in", "w_branch", "w_out", "ffn_w_gate", "ffn_w_up", "ffn_w_down", "moe_w_gate", "moe_w_up", "moe_w_down")

    def din(name, shape):
        if lite and name in BIGW:
            shape = [1] * len(shape)
        return nc.dram_tensor(name, list(shape), F32, kind="ExternalInput").ap()

    x = din("x", [T, DM])
    w_in = din("w_in", [depth, DM, INW])
    w_branch = din("w_branch", [depth, 4 * 512, DM])
    w_out = din("w_out", [depth, DM, DM])
    norm_mix = din("norm_mix", [depth, DM])
    norm_ffn = din("norm_ffn", [depth, DM])
    norm_final = din("norm_final", [1, DM])
    sinks = din("sinks", [depth, 8])
    mla_q_norm = din("mla_q_norm", [depth, 384])
    mla_kv_norm = din("mla_kv_norm", [depth, 256])
    mla_w_uq = din("mla_w_uq", [depth, 384, 768])
    mla_w_ukv = din("mla_w_ukv", [depth, 256, 1024])
    ffn_w_gate = din("ffn_w_gate", [1, DM, DFF])
    ffn_w_up = din("ffn_w_up", [1, DM, DFF])
    ffn_w_down = din("ffn_w_down", [1, DFF, DM])
    router_w = din("router_w", [1, DM, NE])
    moe_w_gate = din("moe_w_gate", [NE, DM, DFE])
    moe_w_up = din("moe_w_up", [NE, DM, DFE])
    moe_w_down = din("moe_w_down", [NE, DFE, DM])
    cst_d = din("cst", [128, NCST, 128])
    tab_d = din("tab", [128, 6, T])
    y = nc.dram_tensor("y", [T, DM], F32, kind="ExternalOutput").ap()
    dbg_out = {}

    h_d = nc.dram_tensor("h_d", [T, DM], F32).ap()
    qT_d = nc.dram_tensor("qT_d", [QROWS, T], BF16).ap()
    K_CH = [(0, 1024), (1024, 2048), (2048, KROWS)]
    V_CH = [(0, 3), (3, 6), (6, 8)]
    kin_c = [nc.dram_tensor("kin%d" % j, [b - a, T], BF16).ap() for j, (a, b) in enumerate(K_CH)]
    kout_c = [nc.dram_tensor("kout%d" % j, [2 * (b - a), T], BF16).ap() for j, (a, b) in enumerate(K_CH)]
    vin_c = [nc.dram_tensor("vin%d" % j, [(b - a) * 128, VCOLS], BF16).ap() for j, (a, b) in enumerate(V_CH)]
    vout_c = [nc.dram_tensor("vout%d" % j, [2 * (b - a) * 128, VCOLS], BF16).ap() for j, (a, b) in enumerate(V_CH)]

    def kin_rows(r0, m):
        for j, (a, b) in enumerate(K_CH):
            if a <= r0 and r0 + m <= b:
                return kin_c[j][r0 - a:r0 - a + m, :]
        raise AssertionError((r0, m))

    def kout_rows(r, r0, m):
        for j, (a, b) in enumerate(K_CH):
            if a <= r0 and r0 + m <= b:
                o = r * (b - a) + r0 - a
                return kout_c[j][o:o + m, :]
        raise AssertionError((r0, m))

    def vin_blk(i):
        for j, (a, b) in enumerate(V_CH):
            if a <= i < b:
                return vin_c[j][(i - a) * 128:(i - a + 1) * 128, :]

    def vout_blk(r, i):
        for j, (a, b) in enumerate(V_CH):
            if a <= i < b:
                o = r * (b - a) * 128 + (i - a) * 128
                return vout_c[j][o:o + 128, :]

    def gd_blk(i):
        return g_d[i * 128:(i + 1) * 128, :]

    def qdst(r0):
        return lambda m, tk: qT_d[r0:r0 + m, tk]

    def kdst(r0):
        return lambda m, tk: kin_rows(r0, m)[:, tk]
    g_d = nc.dram_tensor("g_d", [T, 4 * DM], BF16).ap()
    obr_d = nc.dram_tensor("obr_d", [T, 2048], BF16).ap()
    mT_d = nc.dram_tensor("mT_d", [DM, T], BF16).ap()
    D_obr, D_mT = Buf(None), Buf(None)
    D_h, D_q, D_kin, D_kout, D_vin, D_vout, D_g = (Buf(None) for _ in range(7))

    top = ExitStack()
    cst = kb.sb(top, [128, NCST, 128], BF16, "cst")
    identf = kb.sb(top, [128, 128], F32, "identf")
    kb.dma("pool", cst.t[:], cst_d, writes=[cst])
    kb.dma("sp", identf.t[:], cst_d[:, C_ID, :], writes=[identf])

    def cm(idx, m=128):
        return cst.t[0:m, idx, 0:m]

    def rms_to_T(st, src, gB, nT, i, rotp, tmp, router=None):
        srcbuf, srcap = src
        junk, ss, rstd, nb = tmp
        kb.op("dve", lambda e: e.memset(ss.t[:], 0.0), writes=[ss])
        kb.op("act", lambda e: e.activation(out=junk.t[:], in_=srcap, func=AF.Square, accum_out=ss.t[:]),
              reads=[srcbuf], writes=[junk, ss])
        ck("n2")
        kb.op("act", lambda e: e.activation(out=rstd.t[:], in_=ss.t[:], func=AF.Ln, scale=1.0 / DM, bias=EPS), reads=[ss], writes=[rstd])
        kb.op("act", lambda e: e.activation(out=rstd.t[:], in_=rstd.t[:], func=AF.Exp, scale=-0.5), reads=[rstd], writes=[rstd])
        if router is not None:
            nf = router["nf"]
            kb.op("dve", lambda e: e.scalar_tensor_tensor(out=nf.t[:], in0=srcap, scalar=rstd.t[:, 0:1], in1=gB.t[:],
                                                           op0=ALU.mult, op1=ALU.mult),
                  reads=[srcbuf, rstd, gB], writes=[nf])
            kb.op("act", lambda e: e.copy(out=nb.t[:], in_=nf.t[:]), reads=[nf], writes=[nb])
        else:
            kb.op("dve", lambda e: e.scalar_tensor_tensor(out=nb.t[:], in0=srcap, scalar=rstd.t[:, 0:1], in1=gB.t[:],
                                                           op0=ALU.mult, op1=ALU.mult),
                  reads=[srcbuf, rstd, gB], writes=[nb])
        ck("n3")
        for k0 in range(0, KC, 4):
            n4 = min(4, KC - k0)
            pT = rotp.next()
            for j in range(n4):
                kb.op("pe", lambda e, j=j: e.transpose(out=pT.t[:, j, :], in_=nb.t[:, (k0 + j) * 128:(k0 + j + 1) * 128],
                                                       identity=cm(C_ID)),
                      reads=[nb, cst], writes=[pT], sig=(j == n4 - 1))
            kb.op("act", lambda e: e.copy(out=nT.t[:, k0:k0 + n4, i * 128:(i + 1) * 128], in_=pT.t[:, 0:n4, :]),
                  reads=[pT], writes=[nT])

    def load_gvec(st, src_row, n, name):
        g = kb.sb(st, [128, n], F32, name)
        kb.dma("sp", g.t[:], src_row.partition_broadcast(128), writes=[g])
        return g

    dbg0 = dbg

    def ck(name):
        if dbg == name:
            raise _Stop()

    try:
      for l in range(depth):
        h_src = x if l == 0 else h_d
        with ExitStack() as st:
            nT = kb.sb(st, [128, KC, T], BF16, "nT")
            tab = kb.sb(st, [128, 6, T], F32, "tab")
            kb.dma("sp", tab.t[:], tab_d, writes=[tab])
            gmix = load_gvec(st, norm_mix[l:l + 1, :], DM, "gmix")
            gq = load_gvec(st, mla_q_norm[l:l + 1, :], 384, "gq")
            gkv = load_gvec(st, mla_kv_norm[l:l + 1, :], 256, "gkv")
            cqT = kb.sb(st, [128, 3, T], BF16, "cqT")
            ckvT = kb.sb(st, [128, 2, T], BF16, "ckvT")
            rotT = Rot([kb.ps(st, [128, 4, 128], BF16, "pT")])
            with ExitStack() as st2:
                hb = Rot([kb.sb(st2, [128, DM], F32, "hb") for _ in range(2)])
                tmp = (kb.sb(st2, [128, DM], F32, "junk"), kb.sb(st2, [128, 1], F32, "ss"),
                       kb.sb(st2, [128, 1], F32, "rstd"), kb.sb(st2, [128, DM], BF16, "nb"))
                ck("n0")
                for i in range(NB):
                    b = hb.next()
                    kb.dma("sp", b.t[:], h_src[i * 128:(i + 1) * 128, :], reads=[D_h], writes=[b])
                    ck("n1")
                    rms_to_T(st2, (b, b.t[:]), gmix, nT, i, rotT, tmp)
                    ck("n5")
            ck("n")
            wt = Rot([kb.sb(st, [128, KC, 512], BF16, "wt") for _ in range(3)])
            ps1 = Rot([kb.ps(st, [128, 512], F32, "ps1") for _ in range(2)])
            ps2 = Rot([kb.ps(st, [128, 512], F32, "ps2") for _ in range(2)])
            xb = Rot([kb.sb(st, [128, 512], BF16, "xb") for _ in range(2)])
            t1 = Rot([kb.sb(st, [128, 512], F32, "t1") for _ in range(2)])
            t2 = Rot([kb.sb(st, [128, 512], F32, "t2") for _ in range(2)])
            ob = Rot([kb.sb(st, [128, 512], BF16, "ob") for _ in range(3)])
            sm = (kb.sb(st, [128, 512], F32, "junk2"), kb.sb(st, [128, 1], F32, "ss2"), kb.sb(st, [128, 1], F32, "rstd2"))

            def load_w(c0, n):
                w = wt.next()
                kb.dma("pool", w.t[:, :, 0:n], w_in[l, :, c0:c0 + n].rearrange("(k p) c -> p k c", p=128), writes=[w])
                return w

            def rope_out(p1, m, perm_idx, ci, si, th, scale, dst, Dbuf):
                tk = slice(th * 512, (th + 1) * 512)
                o = ob.next()
                if perm_idx is None:
                    kb.op("act", lambda e: e.activation(out=o.t[0:m, :], in_=p1.t[0:m, :], func=AF.Copy, scale=scale),
                          reads=[p1], writes=[o])
                else:
                    xx, p2, a1, a2 = xb.next(), ps2.next(), t1.next(), t2.next()
                    kb.op("act", lambda e: e.copy(out=xx.t[0:m, :], in_=p1.t[0:m, :]), reads=[p1], writes=[xx])
                    ck("f3")
                    kb.op("pe", lambda e: e.matmul(p2.t[0:m, :], cm(perm_idx, m), xx.t[0:m, :], start=True, stop=True),
                          reads=[xx, cst], writes=[p2])
                    ck("f4")
                    kb.op("dve", lambda e: e.tensor_tensor(out=a1.t[0:m, :], in0=p1.t[0:m, :], in1=tab.t[0:m, ci, tk],
                                                           op=ALU.mult), reads=[p1, tab], writes=[a1])
                    ck("f5a")
                    kb.op("dve", lambda e: e.tensor_tensor(out=a2.t[0:m, :], in0=p2.t[0:m, :], in1=tab.t[0:m, si, tk],
                                                           op=ALU.mult), reads=[p2, tab], writes=[a2])
                    ck("f5b")
                    kb.op("dve", lambda e: e.tensor_tensor(out=o.t[0:m, :], in0=a1.t[0:m, :], in1=a2.t[0:m, :],
                                                           op=ALU.add), reads=[a1, a2], writes=[o])
                    ck("f5")
                for df in dst:
                    kb.dma("sp", df(m, tk), o.t[0:m, :], reads=[o], writes=[Dbuf])

            def formB(c0, ncols, rope, scale, dst_ap, Dbuf, r0):
                for g0 in range(0, ncols, 512):
                    gn = min(512, ncols - g0)
                    w = load_w(c0 + g0, gn)
                    ck("f1")
                    for cc in range(0, gn, 128):
                        m = min(128, gn - cc)
                        for th in range(2):
                            p1 = ps1.next()
                            for k in range(KC):
                                kb.op("pe", lambda e, k=k: e.matmul(p1.t[0:m, :], w.t[:, k, cc:cc + m],
                                                                    nT.t[:, k, th * 512:(th + 1) * 512],
                                                                    start=(k == 0), stop=(k == KC - 1)),
                                      reads=[w, nT], writes=[p1], sig=(k == KC - 1))
                            ck("f2")
                            if rope == "64":
                                rope_out(p1, m, C_P64, 0, 1, th, scale, [dst_ap(r0 + g0 + cc)], Dbuf)
                            elif rope == "R":
                                rope_out(p1, m, C_PR, 4, 5, th, scale, [dst_ap(KD + 96 * hh + 64) for hh in range(8)], Dbuf)
                            else:
                                rope_out(p1, m, None, 0, 0, th, scale, [dst_ap(r0 + g0 + cc)], Dbuf)

            def formA(c0, ncols, post):
                for g0 in range(0, ncols, 512):
                    gn = min(512, ncols - g0)
                    w = load_w(c0 + g0, gn)
                    for i in range(NB):
                        p1 = ps1.next()
                        for k in range(KC):
                            kb.op("pe", lambda e, k=k: e.matmul(p1.t[:, 0:gn], nT.t[:, k, i * 128:(i + 1) * 128],
                                                                w.t[:, k, 0:gn], start=(k == 0), stop=(k == KC - 1)),
                                  reads=[w, nT], writes=[p1], sig=(k == KC - 1))
                        post(p1, g0, gn, i)

            def post_copy(dst_ap, Dbuf, col0, func):
                def f(p1, g0, gn, i):
                    o = ob.next()
                    kb.op("act", lambda e: e.activation(out=o.t[:, 0:gn], in_=p1.t[:, 0:gn], func=func),
                          reads=[p1], writes=[o])
                    kb.dma("sp", dst_ap(i)[:, col0 + g0:col0 + g0 + gn], o.t[:, 0:gn],
                           reads=[o], writes=[Dbuf])
                return f

            def post_mla(gvec, n, dstT):
                def f(p1, g0, gn, i):
                    junk, ss, rstd = sm
                    o = ob.next()
                    kb.op("dve", lambda e: e.memset(ss.t[:], 0.0), writes=[ss])
                    kb.op("act", lambda e: e.activation(out=junk.t[:, 0:n], in_=p1.t[:, 0:n], func=AF.Square,
                                                        accum_out=ss.t[:]), reads=[p1], writes=[junk, ss])
                    kb.op("act", lambda e: e.activation(out=rstd.t[:], in_=ss.t[:], func=AF.Ln, scale=1.0 / n, bias=EPS), reads=[ss], writes=[rstd])
                    kb.op("act", lambda e: e.activation(out=rstd.t[:], in_=rstd.t[:], func=AF.Exp, scale=-0.5), reads=[rstd], writes=[rstd])
                    kb.op("dve", lambda e: e.scalar_tensor_tensor(out=o.t[:, 0:n], in0=p1.t[:, 0:n], scalar=rstd.t[:, 0:1],
                                                                   in1=gvec.t[:, 0:n], op0=ALU.mult, op1=ALU.mult),
                          reads=[p1, rstd, gvec], writes=[o])
                    nch = n // 128
                    pT = rotT.next()
                    for j in range(nch):
                        kb.op("pe", lambda e, j=j: e.transpose(out=pT.t[:, j, :], in_=o.t[:, j * 128:(j + 1) * 128],
                                                               identity=cm(C_ID)),
                              reads=[o, cst], writes=[pT], sig=(j == nch - 1))
                    kb.op("act", lambda e: e.copy(out=dstT.t[:, 0:nch, i * 128:(i + 1) * 128], in_=pT.t[:, 0:nch, :]),
                          reads=[pT], writes=[dstT])
                return f

            kb.barrier()
            formB(W_AQ, 1536, "64", 1.0, qdst, D_q, QA)
            ck("pB")
            formB(W_AK, 1536, "64", 1.0, kdst, D_kin, KA)
            formB(W_BQ, 512, "64", 1.0, qdst, D_q, QB)
            formB(W_BK, 128, "64", 1.0, kdst, D_kin, KB_)
            formB(W_CQ, 512, None, 0.125, qdst, D_q, QC)
            formB(W_CK, 512, None, 1.0, kdst, D_kin, KC_)
            formB(W_DKR, 32, "R", 1.0, kdst, D_kin, 0)
            ck("pB2")
            formA(W_AV, 1536, post_copy(vin_blk, D_vin, VA, AF.Copy))
            formA(W_BV, 128, post_copy(vin_blk, D_vin, VB, AF.Copy))
            formA(W_CV, 512, post_copy(vin_blk, D_vin, VC, AF.Copy))
            ck("pA")
            formA(W_DCQ, 384, post_mla(gq, 384, cqT))
            formA(W_DCKV, 256, post_mla(gkv, 256, ckvT))
            formA(W_G, 4 * DM, post_copy(gd_blk, D_g, 0, AF.Sigmoid))

            ck("pM")
            wuq = kb.sb(st, [128, 3, 768], BF16, "wuq")
            wuk = kb.sb(st, [128, 2, 1024], BF16, "wuk")
            wuv = kb.sb(st, [128, 2, 8, 64], BF16, "wuv")
            kb.dma("pool", wuq.t[:], mla_w_uq[l].rearrange("(k p) c -> p k c", p=128), writes=[wuq])
            kb.dma("pool", wuk.t[:], mla_w_ukv[l].rearrange("(k p) c -> p k c", p=128), writes=[wuk])
            for k in range(2):
                kb.dma("pool", wuv.t[:, k, :, :],
                       mla_w_ukv[l, k * 128:(k + 1) * 128, :].rearrange("p (h x) -> p h x", x=128)[:, :, 64:128],
                       writes=[wuv])
            for hh in range(8):
                for th in range(2):
                    tk = slice(th * 512, (th + 1) * 512)
                    p1 = ps1.next()
                    for k in range(3):
                        kb.op("pe", lambda e, k=k: e.matmul(p1.t[0:96, :], wuq.t[:, k, hh * 96:(hh + 1) * 96], cqT.t[:, k, tk],
                                                            start=(k == 0), stop=(k == 2)),
                              reads=[wuq, cqT], writes=[p1], sig=(k == 2))
                    rope_out(p1, 96, C_PD, 2, 3, th, 1.0, [qdst(QD + 96 * hh)], D_q)
                    p1 = ps1.next()
                    for k in range(2):
                        kb.op("pe", lambda e, k=k: e.matmul(p1.t[0:64, :], wuk.t[:, k, hh * 128:hh * 128 + 64], ckvT.t[:, k, tk],
                                                            start=(k == 0), stop=(k == 1)),
                              reads=[wuk, ckvT], writes=[p1], sig=(k == 1))
                    rope_out(p1, 64, None, 0, 0, th, 1.0, [kdst(KD + 96 * hh)], D_kin)
            for i in range(NB):
                p1 = ps1.next()
                for k in range(2):
                    kb.op("pe", lambda e, k=k: e.matmul(p1.t[:, :], ckvT.t[:, k, i * 128:(i + 1) * 128],
                                                        wuv.t[:, k, :, :].rearrange("p h d -> p (h d)"),
                                                        start=(k == 0), stop=(k == 1)),
                          reads=[wuv, ckvT], writes=[p1], sig=(k == 1))
                post_copy(vin_blk, D_vin, VD, AF.Copy)(p1, 0, 512, i)
            kb.barrier()
        ck("p1x")
        if dbg == "p1" and l == 0:
            o = nc.dram_tensor("d_q0", [QROWS, T], BF16, kind="ExternalOutput").ap()
            for r0 in range(0, QROWS, 128):
                kb.dma("sp", o[r0:r0 + 128, :], qT_d[r0:r0 + 128, :], reads=[D_q], writes=[D_h])
        for j in range(len(K_CH)):
            kb.allgather(kin_c[j], kout_c[j], reads=[D_kin], writes=[D_kout])
        for j in range(len(V_CH)):
            kb.allgather(vin_c[j], vout_c[j], reads=[D_vin], writes=[D_vout])
        if dbg == "p1" and l == 0:
            break

        with ExitStack() as st23:
            with ExitStack() as st:
                obr = kb.sb(st, [128, NB, 2048], BF16, "obr")
                sexp = kb.sb(st, [128, 8], F32, "sexp")
                kb.dma("sp", sexp.t[:], sinks[l:l + 1, :].partition_broadcast(128), writes=[sexp])
                kb.op("act", lambda e: e.activation(out=sexp.t[:], in_=sexp.t[:], func=AF.Exp), reads=[sexp], writes=[sexp])
                pss = Rot([kb.ps(st, [128, 512], F32, "pss") for _ in range(2)])
                psb = Rot([kb.ps(st, [128, 512], F32, "psb") for _ in range(2)])
                psO = Rot([kb.ps(st, [128, NB, 128], F32, "psO") for _ in range(2)])
                Pt = Rot([kb.sb(st, [128, 512], BF16, "Pt") for _ in range(3)])
                Et = Rot([kb.sb(st, [128, 512], F32, "Et") for _ in range(2)])
                Ut = Rot([kb.sb(st, [128, 512], BF16, "Ut") for _ in range(2)])
                Rt = kb.sb(st, [128, 512], BF16, "Rt")
                den = Rot([kb.sb(st, [128, NB, 1], F32, "den") for _ in range(2)])

                def load_mixer(stm, rows, nck, krow0, nh_v, vcol0, ncq, qrow0, bq=False):
                    KT = kb.sb(stm, [128, nck, 16, 128], BF16, "KT")
                    for c in range(nck):
                        for r in range(2):
                            src = kout_rows(r, krow0 + c * rows, rows)
                            kb.dma("sp", KT.t[0:rows, c, :, :].rearrange("p (i r) t -> p i r t", r=2)[:, :, r, :],
                                   src.rearrange("p (i t) -> p i t", t=128), reads=[D_kout], writes=[KT])
                    V = kb.sb(stm, [128, 16, nh_v, 65], BF16, "V")
                    kb.op("pool", lambda e: e.memset(V.t[:, :, :, 64:65], 1.0), writes=[V])
                    for g in range(16):
                        r, i = g % 2, g // 2
                        kb.dma("sp", V.t[:, g, :, 0:64],
                               vout_blk(r, i)[:, vcol0:vcol0 + nh_v * 64].rearrange(
                                   "t (h d) -> t h d", d=64), reads=[D_vout], writes=[V])
                    QT = kb.sb(stm, [128, ncq, T], BF16, "QT")
                    if bq:
                        for h in range(8):
                            kvh = h // 4
                            kb.dma("sp", QT.t[kvh * 64:(kvh + 1) * 64, h % 4, :], qT_d[qrow0 + 64 * h: qrow0 + 64 * (h + 1), :],
                                   reads=[D_q], writes=[QT])
                    else:
                        kb.dma("sp", QT.t[0:rows, :, :],
                               qT_d[qrow0:qrow0 + ncq * rows, :].rearrange("(c p) n -> p c n", p=rows),
                               reads=[D_q], writes=[QT])
                    return KT, V, QT

                def head_ap(Tt, dk, hidx):
                    if dk == 96:
                        return hidx, slice(0, 96)
                    return hidx // 2, slice((hidx % 2) * 64, (hidx % 2) * 64 + 64)

                def full_units(kind):
                    us = []
                    for kbk in range(16):
                        ilo = kbk // 2
                        blocks = list(range(ilo, NB))
                        j = kbk - 2 * ilo
                        for s0 in range(0, len(blocks), 4):
                            bl = blocks[s0:s0 + 4]
                            masks = {}
                            if kind == "A2":
                                for i in bl:
                                    masks[i] = MIDX["A2f"]
                            if ilo in bl:
                                masks[ilo] = MIDX["%s%d" % (kind, j)]
                            us.append((kbk, bl, masks))
                    return us

                def band_units(kind, offs):
                    us = []
                    for i in range(NB):
                        for j in offs:
                            if 2 * i + j < 0:
                                continue
                            us.append((2 * i + j, [i], {i: MIDX["%s%d" % (kind, j)]}))
                    return us

                def run_softmax_head(parts, mixer, h, sink_h=None):
                    O = psO.next()
                    kb.op("dve", lambda e: e.memset(O.t[:], 0.0), writes=[O])
                    total = {}
                    for p in parts:
                        for (kbk, bl, masks) in p[8]:
                            for i in bl:
                                total[i] = total.get(i, 0) + 1
                    seen = {}
                    for (KT, kch, kps, QT, qch, qps, V, vh, units, scale) in parts:
                        for (kbk, bl, masks) in units:
                            n = 128 * len(bl)
                            c0 = bl[0] * 128
                            s_ps = pss.next()
                            kb.op("pe", lambda e: e.matmul(s_ps.t[:, 0:n], KT.t[kps, kch, kbk, :], QT.t[qps, qch, c0:c0 + n],
                                                           start=True, stop=True), reads=[KT, QT], writes=[s_ps])
                            P = Pt.next()
                            kb.op("act", lambda e: e.activation(out=P.t[:, 0:n], in_=s_ps.t[:, 0:n], func=AF.Exp, scale=scale),
                                  reads=[s_ps], writes=[P])
                            mi = sorted(masks.items())
                            if mi and all(m == mi[0][1] for _, m in mi) and len(mi) == len(bl) and len(bl) > 1:
                                midx = mi[0][1]
                                kb.op("dve", lambda e: e.tensor_tensor(
                                    out=P.t[:, 0:n].rearrange("p (b q) -> p b q", q=128),
                                    in0=P.t[:, 0:n].rearrange("p (b q) -> p b q", q=128),
                                    in1=cst.t[:, midx:midx + 1, :].broadcast_to([128, len(bl), 128]), op=ALU.mult),
                                    reads=[P, cst], writes=[P])
                            else:
                                for (i, midx) in mi:
                                    o0 = (i - bl[0]) * 128
                                    kb.op("dve", lambda e, o0=o0, midx=midx: e.tensor_tensor(
                                        out=P.t[:, o0:o0 + 128], in0=P.t[:, o0:o0 + 128], in1=cst.t[:, midx, :], op=ALU.mult),
                                        reads=[P, cst], writes=[P])
                            for i in bl:
                                o0 = (i - bl[0]) * 128
                                seen[i] = seen.get(i, 0) + 1
                                kb.op("pe", lambda e, i=i, o0=o0: e.matmul(O.t[:, i, 0:65], P.t[:, o0:o0 + 128], V.t[:, kbk, vh, :],
                                                                          start=False, stop=(seen[i] == total[i]), skip_group_check=True),
                                      reads=[P, V], writes=[O])
                    dn = den.next()
                    if sink_h is not None:
                        kb.op("dve", lambda e: e.tensor_scalar(out=dn.t[:], in0=O.t[:, :, 64:65], scalar1=sexp.t[:, sink_h:sink_h + 1],
                                                                scalar2=None, op0=ALU.add), reads=[O, sexp], writes=[dn])
                        kb.op("dve", lambda e: e.reciprocal(out=dn.t[:], in_=dn.t[:]), reads=[dn], writes=[dn])
                    else:
                        kb.op("dve", lambda e: e.reciprocal(out=dn.t[:], in_=O.t[:, :, 64:65]), reads=[O], writes=[dn])
                    kb.op("dve", lambda e: e.tensor_tensor(out=obr.t[:, :, mixer * 512 + h * 64: mixer * 512 + h * 64 + 64],
                                                           in0=O.t[:, :, 0:64], in1=dn.t[:].broadcast_to([128, NB, 64]),
                                                           op=ALU.mult), reads=[O, dn], writes=[obr])

                with ExitStack() as stm:
                    KT, V, QT = load_mixer(stm, 128, 12, KA, 24, VA, 12, QA)
                    uA = [band_units("A0", (-1, 0, 1)), band_units("A1", (-4, -3, -2, -1, 0, 1)), full_units("A2")]
                    for h in range(8):
                        parts = []
                        for g in range(3):
                            hh = g * 8 + h
                            ch, psl = head_ap(KT, 64, hh)
                            parts.append((KT, ch, psl, QT, ch, psl, V, hh, uA[g], 0.125))
                        run_softmax_head(parts, 0, h)
                    kb.barrier()
                with ExitStack() as stm:
                    KT, V, QT = load_mixer(stm, 128, 1, KB_, 2, VB, 4, QB, bq=True)
                    uB = band_units("B", (-1, 0, 1))
                    for h in range(8):
                        kvh = h // 4
                        kps = slice(kvh * 64, kvh * 64 + 64)
                        run_softmax_head([(KT, 0, kps, QT, h % 4, kps, V, kvh, uB, 0.125)], 1, h, sink_h=h)
                    kb.barrier()
                with ExitStack() as stm:
                    KT, V, QT = load_mixer(stm, 96, 8, KD, 8, VD, 8, QD)
                    uD = full_units("D")
                    for h in range(8):
                        run_softmax_head([(KT, h, slice(0, 96), QT, h, slice(0, 96), V, h, uD, 96 ** -0.5)], 3, h)
                    kb.barrier()
                with ExitStack() as stm:
                    KT, V, QT = load_mixer(stm, 128, 4, KC_, 8, VC, 4, QC)
                    for h in range(8):
                        ch, psl = head_ap(KT, 64, h)
                        for i0 in (0, 4):
                            O = psO.next()
                            kb.op("dve", lambda e: e.memset(O.t[:], 0.0), writes=[O])
                            first = True
                            seen = {}
                            kmax = 2 * (i0 + 3) + 1
                            for kbk in range(kmax, -1, -1):
                                ilo = max(kbk // 2, i0)
                                bl = list(range(ilo, i0 + 4))
                                n = 128 * len(bl)
                                c0 = ilo * 128
                                l0 = (ilo - i0) * 128
                                bmask = MIDX["C%d" % (kbk - 2 * (kbk // 2))] if kbk // 2 >= i0 else None
                                pa = pss.next()
                                kb.op("pe", lambda e: e.matmul(pa.t[:, 0:n], KT.t[psl, ch, kbk, :], QT.t[psl, ch, c0:c0 + n],
                                                               start=True, stop=True), reads=[KT, QT], writes=[pa])
                                E = Et.next()
                                U = Ut.next()
                                kb.op("act", lambda e: e.activation(out=E.t[:, 0:n], in_=pa.t[:, 0:n], func=AF.Exp),
                                      reads=[pa], writes=[E])
                                kb.op("act", lambda e: e.activation(out=U.t[:, 0:n], in_=E.t[:, 0:n], func=AF.Ln, bias=1.0),
                                      reads=[E], writes=[U])
                                if bmask is not None:
                                    kb.op("dve", lambda e: e.tensor_tensor(out=U.t[:, 0:128], in0=U.t[:, 0:128],
                                                                           in1=cst.t[:, bmask, :], op=ALU.mult),
                                          reads=[U, cst], writes=[U])
                                pb = psb.next()
                                kb.op("pe", lambda e: e.matmul(pb.t[:, 0:n], KT.t[psl, ch, kbk, :], QT.t[psl, ch, c0:c0 + n],
                                                               start=True, stop=False), reads=[KT, QT], writes=[pb], sig=False)
                                kb.op("pe", lambda e: e.matmul(pb.t[:, 0:n], cm(C_TRI), U.t[:, 0:n], start=False, stop=first),
                                      reads=[cst, U], writes=[pb], sig=first)
                                if not first:
                                    kb.op("pe", lambda e: e.matmul(pb.t[:, 0:n], cm(C_ONES), Rt.t[:, l0:l0 + n], start=False, stop=True),
                                          reads=[cst, Rt], writes=[pb])
                                P = Pt.next()
                                kb.op("act", lambda e: e.activation(out=P.t[:, 0:n], in_=pb.t[:, 0:n], func=AF.Exp),
                                      reads=[pb], writes=[P])
                                if bmask is not None:
                                    kb.op("dve", lambda e: e.tensor_tensor(out=P.t[:, 0:128], in0=P.t[:, 0:128],
                                                                           in1=cst.t[:, bmask, :], op=ALU.mult),
                                          reads=[P, cst], writes=[P])
                                for i in bl:
                                    o0 = (i - ilo) * 128
                                    nvis = 2 * i + 2
                                    seen[i] = seen.get(i, 0) + 1
                                    kb.op("pe", lambda e, i=i, o0=o0: e.matmul(O.t[:, i - i0, 0:64], P.t[:, o0:o0 + 128], V.t[:, kbk, h, 0:64],
                                                                              start=False, stop=(seen[i] == nvis), skip_group_check=True),
                                          reads=[P, V], writes=[O])
                                if kbk > 0:
                                    if first:
                                        kb.op("dve", lambda e: e.memset(Rt.t[:], 0.0), writes=[Rt])
                                    kb.op("dve", lambda e: e.tensor_tensor(out=Rt.t[:, l0:l0 + n], in0=Rt.t[:, l0:l0 + n],
                                                                           in1=U.t[:, 0:n], op=ALU.add),
                                          reads=[Rt, U], writes=[Rt])
                                first = False
                            kb.op("act", lambda e: e.copy(out=obr.t[:, i0:i0 + 4, 2 * 512 + h * 64: 2 * 512 + h * 64 + 64],
                                                          in_=O.t[:, 0:4, 0:64]), reads=[O], writes=[obr])
                    kb.barrier()
                for i in range(NB):
                    kb.dma("sp", obr_d[i * 128:(i + 1) * 128, :], obr.t[:, i, :], reads=[obr], writes=[D_obr])
                kb.barrier()

            with ExitStack() as st3:
                with ExitStack() as st:
                    wb = kb.sb(st, [128, 16, DM], BF16, "wb")
                    kb.dma("pool", wb.t[:], w_branch[l].rearrange("(c p) d -> p c d", p=128), writes=[wb])
                    oT = Rot([kb.sb(st, [128, 16, 128], BF16, "oT") for _ in range(2)])
                    obk = Rot([kb.sb(st, [128, 2048], BF16, "obk") for _ in range(2)])
                    mTb = Rot([kb.sb(st, [128, KC, 128], BF16, "mTb") for _ in range(2)])
                    gt = Rot([kb.sb(st, [128, 4 * DM], BF16, "gt") for _ in range(2)])
                    mg = Rot([kb.sb(st, [128, DM], F32, "mg") for _ in range(2)])
                    mb = Rot([kb.sb(st, [128, DM], BF16, "mb") for _ in range(2)])
                    tm = Rot([kb.sb(st, [128, CG], F32, "tm") for _ in range(2)])
                    rotT = Rot([kb.ps(st, [128, 4, 128], BF16, "pT") for _ in range(2)])
                    psm = Rot([kb.ps(st, [128, CG], F32, "psm") for _ in range(3)])
                    for i in range(NB):
                        o_t, g_t, m_g, m_b = oT.next(), gt.next(), mg.next(), mb.next()
                        ob_, mT_ = obk.next(), mTb.next()
                        kb.dma("sp", ob_.t[:], obr_d[i * 128:(i + 1) * 128, :], reads=[D_obr], writes=[ob_])
                        kb.dma("sp", g_t.t[:], g_d[i * 128:(i + 1) * 128, :], reads=[D_g], writes=[g_t])
                        for c0 in range(0, 16, 4):
                            pT = rotT.next()
                            for j in range(4):
                                kb.op("pe", lambda e, j=j: e.transpose(out=pT.t[:, j, :], in_=ob_.t[:, (c0 + j) * 128:(c0 + j + 1) * 128],
                                                                       identity=cm(C_ID)), reads=[ob_, cst], writes=[pT], sig=(j == 3))
                            kb.op("act", lambda e: e.copy(out=o_t.t[:, c0:c0 + 4, :], in_=pT.t[:]), reads=[pT], writes=[o_t])
                        for n_ in range(4):
                            for cg in range(NCG):
                                pm = psm.next()
                                for c in range(4):
                                    kb.op("pe", lambda e, c=c: e.matmul(pm.t[:], o_t.t[:, 4 * n_ + c, :], wb.t[:, 4 * n_ + c, cg * CG:(cg + 1) * CG],
                                                                        start=(c == 0), stop=(c == 3)),
                                          reads=[o_t, wb], writes=[pm], sig=(c == 3))
                                gsl = g_t.t[:, n_ * DM + cg * CG: n_ * DM + (cg + 1) * CG]
                                if n_ == 0:
                                    kb.op("dve", lambda e: e.tensor_tensor(out=m_g.t[:, cg * CG:(cg + 1) * CG], in0=pm.t[:], in1=gsl,
                                                                           op=ALU.mult), reads=[pm, g_t], writes=[m_g])
                                else:
                                    tt = tm.next()
                                    kb.op("dve", lambda e: e.tensor_tensor(out=tt.t[:], in0=pm.t[:], in1=gsl, op=ALU.mult),
                                          reads=[pm, g_t], writes=[tt])
                                    kb.op("pool", lambda e: e.tensor_tensor(out=m_g.t[:, cg * CG:(cg + 1) * CG],
                                                                            in0=m_g.t[:, cg * CG:(cg + 1) * CG], in1=tt.t[:], op=ALU.add),
                                          reads=[m_g, tt], writes=[m_g])
                        kb.op("act", lambda e: e.copy(out=m_b.t[:], in_=m_g.t[:]), reads=[m_g], writes=[m_b])
                        for k0 in range(0, KC, 4):
                            n4 = min(4, KC - k0)
                            pT = rotT.next()
                            for j in range(n4):
                                kb.op("pe", lambda e, j=j: e.transpose(out=pT.t[:, j, :], in_=m_b.t[:, (k0 + j) * 128:(k0 + j + 1) * 128],
                                                                       identity=cm(C_ID)), reads=[m_b, cst], writes=[pT], sig=(j == n4 - 1))
                            kb.op("act", lambda e: e.copy(out=mT_.t[:, k0:k0 + n4, :], in_=pT.t[:, 0:n4, :]),
                                  reads=[pT], writes=[mT_])
                        kb.dma("sp", mT_d.rearrange("(k p) n -> p k n", p=128)[:, :, i * 128:(i + 1) * 128], mT_.t[:],
                               reads=[mT_], writes=[D_mT])
                    kb.barrier()
                moe = (l % 2 == 1)
                st4 = ExitStack()
                acc = kb.sb(st4, [128, NB, DM], F32, "acc")
                n2T = kb.sb(st4, [128, KC, T], BF16, "n2T")
                cw = kb.sb(st4, [128, NB, NE], F32, "cw")
                with ExitStack() as st:
                    wo = Rot([kb.sb(st, [128, KC, CG], BF16, "wo") for _ in range(2)])
                    mT = kb.sb(st, [128, KC, T], BF16, "mT")
                    kb.dma("sp", mT.t[:], mT_d.rearrange("(k p) n -> p k n", p=128), reads=[D_mT], writes=[mT])
                    hcg = Rot([kb.sb(st, [128, CG], F32, "hcg") for _ in range(3)])
                    pso = Rot([kb.ps(st, [128, CG], F32, "pso") for _ in range(2)])
                    for cg in range(NCG):
                        w = wo.next()
                        kb.dma("pool", w.t[:], w_out[l, :, cg * CG:(cg + 1) * CG].rearrange("(k p) c -> p k c", p=128), writes=[w])
                        for i in range(NB):
                            hh_ = hcg.next()
                            kb.dma("sp", hh_.t[:], h_src[i * 128:(i + 1) * 128, cg * CG:(cg + 1) * CG], reads=[D_h], writes=[hh_])
                            po = pso.next()
                            for k in range(KC):
                                kb.op("pe", lambda e, k=k: e.matmul(po.t[:], mT.t[:, k, i * 128:(i + 1) * 128], w.t[:, k, :],
                                                                    start=(k == 0), stop=(k == KC - 1)),
                                      reads=[mT, w], writes=[po], sig=(k == KC - 1))
                            kb.op("dve", lambda e: e.tensor_tensor(out=acc.t[:, i, cg * CG:(cg + 1) * CG], in0=po.t[:], in1=hh_.t[:],
                                                                   op=ALU.add), reads=[po, hh_], writes=[acc])
                    kb.barrier()
                with ExitStack() as st:
                    gffn = load_gvec(st, norm_ffn[l:l + 1, :], DM, "gffn")
                    rotT = Rot([kb.ps(st, [128, 4, 128], BF16, "pT") for _ in range(2)])
                    tmp = (kb.sb(st, [128, DM], F32, "junk"), kb.sb(st, [128, 1], F32, "ss"),
                           kb.sb(st, [128, 1], F32, "rstd"), kb.sb(st, [128, DM], BF16, "nb"))
                    router = None
                    if moe:
                        router = dict(nf=kb.sb(st, [128, DM], F32, "nf"))
                        wr = kb.sb(st, [128, KC, NE], F32, "wr")
                        kb.dma("sp", wr.t[:], router_w[0].rearrange("(k p) e -> p k e", p=128), writes=[wr])
                        pTf = Rot([kb.ps(st, [128, 128], F32, "pTf") for _ in range(2)])
                        nfT = Rot([kb.sb(st, [128, 128], F32, "nfT") for _ in range(2)])
                        psl_ = kb.ps(st, [128, NE], F32, "psl")
                        lg = kb.sb(st, [128, NE], F32, "lg")
                        l2 = kb.sb(st, [128, NE], F32, "l2")
                        mk1 = kb.sb(st, [128, NE], F32, "mk1")
                        mk2 = kb.sb(st, [128, NE], F32, "mk2")
                        m1 = kb.sb(st, [128, 1], F32, "m1")
                        m2 = kb.sb(st, [128, 1], F32, "m2")
                        ee = kb.sb(st, [128, 1], F32, "ee")
                        w1 = kb.sb(st, [128, 1], F32, "w1")
                        w2 = kb.sb(st, [128, 1], F32, "w2")
                    for i in range(NB):
                        rms_to_T(st, (acc, acc.t[:, i, :]), gffn, n2T, i, rotT, tmp, router)
                        if moe:
                            nf = router["nf"]
                            for k in range(KC):
                                pf, nt_ = pTf.next(), nfT.next()
                                kb.op("pe", lambda e: e.transpose(out=pf.t[:], in_=nf.t[:, k * 128:(k + 1) * 128], identity=identf.t[:]),
                                      reads=[nf, identf], writes=[pf])
                                kb.op("dve", lambda e: e.tensor_copy(out=nt_.t[:], in_=pf.t[:]), reads=[pf], writes=[nt_])
                                kb.op("pe", lambda e: e.matmul(psl_.t[:], nt_.t[:], wr.t[:, k, :], start=(k == 0), stop=(k == KC - 1)),
                                      reads=[nt_, wr], writes=[psl_])
                            kb.op("dve", lambda e: e.tensor_copy(out=lg.t[:], in_=psl_.t[:]), reads=[psl_], writes=[lg])
                            kb.op("dve", lambda e: e.reduce_max(out=m1.t[:], in_=lg.t[:], axis=mybir.AxisListType.X), reads=[lg], writes=[m1])
                            kb.op("dve", lambda e: e.tensor_scalar(out=mk1.t[:], in0=lg.t[:], scalar1=m1.t[:, 0:1], scalar2=None,
                                                                    op0=ALU.is_ge), reads=[lg, m1], writes=[mk1])
                            kb.op("dve", lambda e: e.scalar_tensor_tensor(out=l2.t[:], in0=mk1.t[:], scalar=-1e30, in1=lg.t[:],
                                                                           op0=ALU.mult, op1=ALU.add), reads=[mk1, lg], writes=[l2])
                            kb.op("dve", lambda e: e.reduce_max(out=m2.t[:], in_=l2.t[:], axis=mybir.AxisListType.X), reads=[l2], writes=[m2])
                            kb.op("dve", lambda e: e.tensor_scalar(out=mk2.t[:], in0=l2.t[:], scalar1=m2.t[:, 0:1], scalar2=None,
                                                                    op0=ALU.is_ge), reads=[l2, m2], writes=[mk2])
                            kb.op("dve", lambda e: e.tensor_tensor(out=ee.t[:], in0=m2.t[:], in1=m1.t[:], op=ALU.subtract),
                                  reads=[m1, m2], writes=[ee])
                            kb.op("act", lambda e: e.activation(out=ee.t[:], in_=ee.t[:], func=AF.Exp), reads=[ee], writes=[ee])
                            kb.op("dve", lambda e: e.tensor_scalar(out=w1.t[:], in0=ee.t[:], scalar1=1.0, scalar2=None, op0=ALU.add),
                                  reads=[ee], writes=[w1])
                            kb.op("dve", lambda e: e.reciprocal(out=w1.t[:], in_=w1.t[:]), reads=[w1], writes=[w1])
                            kb.op("dve", lambda e: e.tensor_tensor(out=w2.t[:], in0=ee.t[:], in1=w1.t[:], op=ALU.mult),
                                  reads=[ee, w1], writes=[w2])
                            kb.op("dve", lambda e: e.tensor_scalar(out=mk1.t[:], in0=mk1.t[:], scalar1=w1.t[:, 0:1], scalar2=None,
                                                                    op0=ALU.mult), reads=[mk1, w1], writes=[mk1])
                            kb.op("dve", lambda e: e.scalar_tensor_tensor(out=cw.t[:, i, :], in0=mk2.t[:], scalar=w2.t[:, 0:1], in1=mk1.t[:],
                                                                           op0=ALU.mult, op1=ALU.add), reads=[mk2, w2, mk1], writes=[cw])
                    kb.barrier()
                with ExitStack() as st:
                    FG = 256
                    wg = Rot([kb.sb(st, [128, KC, FG], BF16, "wg") for _ in range(3)])
                    wu = Rot([kb.sb(st, [128, KC, FG], BF16, "wu") for _ in range(3)])
                    wd = Rot([kb.sb(st, [128, FG // 128, DM], BF16, "wd") for _ in range(3)])
                    hT = Rot([kb.sb(st, [128, FG // 128, T], BF16, "hT") for _ in range(2)])
                    sg = Rot([kb.sb(st, [128, 512], F32, "sg") for _ in range(2)])
                    psg = Rot([kb.ps(st, [128, 512], F32, "psg") for _ in range(2)])
                    psu = Rot([kb.ps(st, [128, 512], F32, "psu") for _ in range(2)])
                    psd = Rot([kb.ps(st, [128, CG], F32, "psd") for _ in range(3)])
                    if moe:
                        experts = [(moe_w_gate[e_], moe_w_up[e_], moe_w_down[e_], DFE, e_) for e_ in range(NE)]
                    else:
                        experts = [(ffn_w_gate[0], ffn_w_up[0], ffn_w_down[0], DFF, None)]
                    for (Wg, Wu, Wd, dff, eidx) in experts:
                        for f0 in range(0, dff, FG):
                            a, b, d, hT_ = wg.next(), wu.next(), wd.next(), hT.next()
                            kb.dma("pool", a.t[:], Wg[:, f0:f0 + FG].rearrange("(k p) c -> p k c", p=128), writes=[a])
                            kb.dma("pool", b.t[:], Wu[:, f0:f0 + FG].rearrange("(k p) c -> p k c", p=128), writes=[b])
                            kb.dma("pool", d.t[:], Wd[f0:f0 + FG, :].rearrange("(c p) d -> p c d", p=128), writes=[d])
                            for fc in range(FG // 128):
                                for th in range(2):
                                    tk = slice(th * 512, (th + 1) * 512)
                                    pg, pu = psg.next(), psu.next()
                                    for k in range(KC):
                                        kb.op("pe", lambda e, k=k: e.matmul(pg.t[:], a.t[:, k, fc * 128:(fc + 1) * 128], n2T.t[:, k, tk],
                                                                            start=(k == 0), stop=(k == KC - 1)),
                                              reads=[a, n2T], writes=[pg], sig=(k == KC - 1))
                                    for k in range(KC):
                                        kb.op("pe", lambda e, k=k: e.matmul(pu.t[:], b.t[:, k, fc * 128:(fc + 1) * 128], n2T.t[:, k, tk],
                                                                            start=(k == 0), stop=(k == KC - 1)),
                                              reads=[b, n2T], writes=[pu], sig=(k == KC - 1))
                                    s_ = sg.next()
                                    kb.op("act", lambda e: e.activation(out=s_.t[:], in_=pg.t[:], func=AF.Silu), reads=[pg], writes=[s_])
                                    kb.op("dve", lambda e: e.tensor_tensor(out=hT_.t[:, fc, tk], in0=pu.t[:], in1=s_.t[:], op=ALU.mult),
                                          reads=[pu, s_], writes=[hT_])
                            for i in range(NB):
                                for cg in range(NCG):
                                    pd = psd.next()
                                    nf_ = FG // 128
                                    for fc in range(nf_):
                                        kb.op("pe", lambda e, fc=fc: e.matmul(pd.t[:], hT_.t[:, fc, i * 128:(i + 1) * 128], d.t[:, fc, cg * CG:(cg + 1) * CG],
                                                                              start=(fc == 0), stop=(fc == nf_ - 1)),
                                              reads=[hT_, d], writes=[pd], sig=(fc == nf_ - 1))
                                    asl = acc.t[:, i, cg * CG:(cg + 1) * CG]
                                    if eidx is None:
                                        kb.op("dve", lambda e: e.tensor_tensor(out=asl, in0=pd.t[:], in1=asl, op=ALU.add),
                                              reads=[pd, acc], writes=[acc])
                                    else:
                                        kb.op("dve", lambda e: e.scalar_tensor_tensor(out=asl, in0=pd.t[:], scalar=cw.t[:, i, eidx:eidx + 1],
                                                                                       in1=asl, op0=ALU.mult, op1=ALU.add),
                                              reads=[pd, acc, cw], writes=[acc])
                    kb.barrier()
                with ExitStack() as st:
                    if l < depth - 1:
                        for i in range(NB):
                            kb.dma("sp", h_d[i * 128:(i + 1) * 128, :], acc.t[:, i, :], reads=[acc], writes=[D_h])
                    else:
                        gfin = load_gvec(st, norm_final[0:1, :], DM, "gfin")
                        junk = kb.sb(st, [128, DM], F32, "junk")
                        ss = kb.sb(st, [128, 1], F32, "ss")
                        rstd = kb.sb(st, [128, 1], F32, "rstd")
                        yo = Rot([kb.sb(st, [128, DM], F32, "yo") for _ in range(2)])
                        for i in range(NB):
                            kb.op("dve", lambda e: e.memset(ss.t[:], 0.0), writes=[ss])
                            kb.op("act", lambda e: e.activation(out=junk.t[:], in_=acc.t[:, i, :], func=AF.Square, accum_out=ss.t[:]),
                                  reads=[acc], writes=[junk, ss])
                            kb.op("act", lambda e: e.activation(out=rstd.t[:], in_=ss.t[:], func=AF.Ln, scale=1.0 / DM, bias=EPS), reads=[ss], writes=[rstd])
                            kb.op("act", lambda e: e.activation(out=rstd.t[:], in_=rstd.t[:], func=AF.Exp, scale=-0.5), reads=[rstd], writes=[rstd])
                            o = yo.next()
                            kb.op("dve", lambda e: e.scalar_tensor_tensor(out=o.t[:], in0=acc.t[:, i, :], scalar=rstd.t[:, 0:1], in1=gfin.t[:],
                                                                           op0=ALU.mult, op1=ALU.mult), reads=[acc, rstd, gfin], writes=[o])
                            kb.dma("sp", y[i * 128:(i + 1) * 128, :], o.t[:], reads=[o], writes=[D_h])
                    kb.barrier()
                st4.close()
                ck("l%d" % l)
    except _Stop:
        dbg = "stopped"
    if dbg0 in ("l0", "l1"):
        for nm, src, shp, dt_ in (("d_obr", obr_d, [T, 2048], BF16), ("d_h", h_d, [T, DM], F32), ("d_mT", mT_d, [DM, T], BF16)):
            o = nc.dram_tensor(nm, shp, dt_, kind="ExternalOutput").ap()
            for r0 in range(0, shp[0], 128):
                kb.dma("sp", o[r0:r0 + 128, :], src[r0:r0 + 128, :], reads=[D_obr, D_mT, D_h], writes=[D_q])
    if dbg == "p1":
        dumps = [("d_q", qT_d, [QROWS, T]), ("d_g", g_d, [T, 4 * DM])]
        dumps += [("d_k%d" % j, kout_c[j], [2 * (b_ - a_), T]) for j, (a_, b_) in enumerate(K_CH)]
        dumps += [("d_v%d" % j, vout_c[j], [2 * (b_ - a_) * 128, VCOLS]) for j, (a_, b_) in enumerate(V_CH)]
        for nm, src, shp in dumps:
            o = nc.dram_tensor(nm, shp, BF16, kind="ExternalOutput").ap()
            for r0 in range(0, shp[0], 128):
                kb.dma("sp", o[r0:r0 + 128, :], src[r0:r0 + 128, :], reads=[D_q, D_kout, D_vout, D_g], writes=[D_h])
    kb.barrier()
    if dbg != "stopped":
        top.close()
    return nc


def _masks(p):
    k = np.arange(128)[:, None]
    q = np.arange(128)[None, :]

    def m(kind, delta):
        d = 128 * delta + q - k
        if kind == "D":
            v = d >= 0
        elif kind == "C":
            v = d >= 1
        elif kind == "B":
            v = (d >= 0) & (d <= 127)
        elif kind == "A0":
            v = (d >= 0) & (d <= 128)
        elif kind == "A1":
            v = (d >= 0) & (d % 4 == 0) & (d <= 512)
        elif kind == "A2":
            v = (d >= 0) & (d % 16 == 0)
        return v.astype(np.float32)

    out = {}
    for j in (0, 1):
        out["D%d" % j] = m("D", p - j)
        out["C%d" % j] = m("C", p - j)
        out["A2%d" % j] = m("A2", p - j)
    out["A2f"] = m("A2", 3)
    for j in (-1, 0, 1):
        out["B%d" % j] = m("B", p - j)
        out["A0%d" % j] = m("A0", p - j)
    for j in (-4, -3, -2, -1, 0, 1):
        out["A1%d" % j] = m("A1", p - j)
    return out


def _consts(p):
    c = np.zeros((128, NCST, 128), np.float32)
    c[:, C_ID, :] = np.eye(128)
    for m_ in range(128):
        d = m_ % 64
        c[(m_ - d) + (d + 32) % 64, C_P64, m_] = 1.0
    for m_ in range(64, 96):
        d = m_ - 64
        c[64 + (d + 16) % 32, C_PD, m_] = 1.0
    for m_ in range(32):
        c[(m_ + 16) % 32, C_PR, m_] = 1.0
    jj = np.arange(128)[:, None]
    ss = np.arange(128)[None, :]
    c[:, C_TRI, :] = -(jj >= ss).astype(np.float32)
    c[:, C_ONES, :] = -1.0
    mk = _masks(p)
    for n in MASK_NAMES:
        c[:, MIDX[n], :] = mk[n]
    return c


def _tables(p):
    pos = (128 * (2 * np.arange(NB)[:, None] + p) + np.arange(128)[None, :]).reshape(-1).astype(np.float32)
    tab = np.zeros((128, 6, T), np.float32)
    f64 = (10000.0 ** (-2.0 * np.arange(32, dtype=np.float32) / 64)).astype(np.float32)
    f32_ = (10000.0 ** (-2.0 * np.arange(16, dtype=np.float32) / 32)).astype(np.float32)
    for r in range(128):
        d = r % 64
        ang = pos * f64[d % 32]
        tab[r, 0] = np.cos(ang)
        tab[r, 1] = np.sin(ang) * (-1.0 if d < 32 else 1.0)
    tab[0:64, 2] = 1.0
    for r in range(32):
        ang = pos * f32_[r % 16]
        sgn = -1.0 if r < 16 else 1.0
        tab[64 + r, 2] = np.cos(ang)
        tab[64 + r, 3] = np.sin(ang) * sgn
        tab[r, 4] = np.cos(ang)
        tab[r, 5] = np.sin(ang) * sgn
    return tab


_NC_CACHE = {}


def make_in_maps(inp):
    f = lambda a: np.ascontiguousarray(np.asarray(a, dtype=np.float32))
    xs = f(inp["x"])
    B, S_, DM = xs.shape
    depth = inp["w_in"].shape[0]
    shared = dict(
        w_in=f(inp["w_in"]), w_branch=f(inp["w_branch"]).reshape(depth, 4 * 512, DM), w_out=f(inp["w_out"]),
        norm_mix=f(inp["norm_mix"]), norm_ffn=f(inp["norm_ffn"]), norm_final=f(inp["norm_final"]).reshape(1, DM),
        sinks=f(inp["sinks"]), mla_q_norm=f(inp["mla_q_norm"]), mla_kv_norm=f(inp["mla_kv_norm"]),
        mla_w_uq=f(inp["mla_w_uq"]), mla_w_ukv=f(inp["mla_w_ukv"]),
        ffn_w_gate=f(inp["ffn_w_gate"]), ffn_w_up=f(inp["ffn_w_up"]), ffn_w_down=f(inp["ffn_w_down"]),
        router_w=f(inp["router_w"]), moe_w_gate=f(inp["moe_w_gate"])[0], moe_w_up=f(inp["moe_w_up"])[0],
        moe_w_down=f(inp["moe_w_down"])[0])
    maps = []
    for c in range(8):
        b, p = c // 2, c % 2
        m = dict(shared)
        m["x"] = np.ascontiguousarray(xs[b].reshape(16, 128, DM)[p::2].reshape(T, DM))
        m["cst"] = _consts(p)
        m["tab"] = _tables(p)
        maps.append(m)
    return maps


def kernel(**inp):
    DM = inp["x"].shape[2]
    DFF = inp["ffn_w_gate"].shape[2]
    DFE = inp["moe_w_gate"].shape[3]
    key = (DM, DFF, DFE)
    if key not in _NC_CACHE:
        _NC_CACHE[key] = build(DM, DFF, DFE)
    nc = _NC_CACHE[key]
    maps = make_in_maps(inp)
    res = run_bass_kernel_spmd(nc, maps, core_ids=list(range(8)))
    out = np.zeros((4, 16, 128, DM), np.float32)
    for c in range(8):
        b, p = c // 2, c % 2
        out[b, p::2] = np.asarray(res.results[c]["y"]).reshape(NB, 128, DM)
    return out.reshape(4, S, DM)
```
